# Optimizing a Trainium2 kernel written in Bass

```python
import math
import jax
import jax.numpy as jnp
from jax import lax
import numpy as np

D_MODEL = 1024
BATCH = 16
SEQ = 4096
DEPTH = 4

GRID_W = 64
CTX_LEN = 256

LRU_WIDTH = 256
LRU_BLOCKS = 4
LRU_BLOCK = LRU_WIDTH // LRU_BLOCKS
CONV_W = 4
LRU_C = 8.0
DA_HEADS = 6
DA_QK = 32
DA_V = 2 * DA_QK
DA_WIDTH = DA_HEADS * DA_V
MLA_HEADS = 6
MLA_NOPE = 64
MLA_ROPE = 32
MLA_V = 64
MLA_WIDTH = MLA_HEADS * MLA_V
Q_RANK = 256
KV_RANK = 128
MLA_SCALE = (MLA_NOPE + MLA_ROPE) ** -0.5

D_MIX = LRU_WIDTH + DA_WIDTH + MLA_WIDTH
IN_SPLITS = (LRU_WIDTH, LRU_WIDTH, DA_HEADS * 2 * DA_QK, DA_HEADS * 2 * DA_QK, DA_WIDTH, Q_RANK, KV_RANK, MLA_ROPE)
D_IN = sum(IN_SPLITS)

N_EXPERTS = 16
N_GROUPS = 4
EXPERTS_PER_GROUP = N_EXPERTS // N_GROUPS
TOP_K = 2
D_EXPERT = 256

ROPE_THETA = 10000.0
Q_BLOCK = 128
LN_EPS = 1e-5
RMS_EPS = 1e-6
DEEPNORM_ALPHA = (2 * DEPTH) ** 0.25
DEEPNORM_BETA = (8 * DEPTH) ** -0.25

kernel_name = "hybrid_lru_diffattn_mla_moe_dit"


def layer_norm(x, g, b):
    xf = x.astype(jnp.float32)
    mu = jnp.mean(xf, axis=-1, keepdims=True)
    var = jnp.mean(jnp.square(xf - mu), axis=-1, keepdims=True)
    return ((xf - mu) * lax.rsqrt(var + LN_EPS)).astype(x.dtype) * g + b


def rms_norm(x, g):
    xf = x.astype(jnp.float32)
    return (xf * lax.rsqrt(jnp.mean(xf * xf, axis=-1, keepdims=True) + RMS_EPS)).astype(x.dtype) * g


def axial_rotary(n, dim):
    rows = n // GRID_W
    row = jnp.repeat(jnp.arange(rows), GRID_W).astype(jnp.float32)
    col = jnp.tile(jnp.arange(GRID_W), rows).astype(jnp.float32)
    n_freq = dim // 4
    inv = ROPE_THETA ** (-jnp.arange(n_freq, dtype=jnp.float32) / n_freq)
    ang = jnp.concatenate([row[:, None] * inv, col[:, None] * inv], axis=-1)
    return jnp.cos(ang), jnp.sin(ang)


def apply_rotary(t, cos, sin):
    t1, t2 = jnp.split(t, 2, axis=-1)
    cos = cos.astype(t.dtype)
    sin = sin.astype(t.dtype)
    return jnp.concatenate([t1 * cos - t2 * sin, t1 * sin + t2 * cos], axis=-1)


def split_in(p):
    idx = [int(v) for v in np.cumsum(IN_SPLITS)[:-1]]
    return jnp.split(p, idx, axis=-1)


def sweep_query_blocks(fn, *qs):
    bsz, h, s, _ = qs[0].shape
    nb = s // Q_BLOCK
    blocks = tuple(q.reshape(bsz, h, nb, Q_BLOCK, q.shape[-1]).transpose(2, 0, 1, 3, 4) for q in qs)
    out = lax.map(lambda blk: fn(*blk), blocks)
    return out.transpose(1, 2, 0, 3, 4).reshape(bsz, h, s, out.shape[-1])


def short_conv(x, w, b):
    n = x.shape[1]
    left = CONV_W // 2
    xp = jnp.pad(x, ((0, 0), (left, CONV_W - 1 - left), (0, 0)))
    y = b + xp[:, 0:n] * w[0]
    for k in range(1, CONV_W):
        y = y + xp[:, k:k + n] * w[k]
    return y


def lru_coeffs(y, wa, ba, wi, bi, lam):
    bsz, n, _ = y.shape
    yb = y.reshape(bsz, n, LRU_BLOCKS, LRU_BLOCK)
    r = jax.nn.sigmoid(jnp.einsum('bnhi,hij->bnhj', yb, wa).reshape(bsz, n, LRU_WIDTH) + ba)
    i = jax.nn.sigmoid(jnp.einsum('bnhi,hij->bnhj', yb, wi).reshape(bsz, n, LRU_WIDTH) + bi)
    log_a = -LRU_C * jax.nn.softplus(-lam.astype(jnp.float32)) * r.astype(jnp.float32)
    a = jnp.exp(log_a)
    u = jnp.sqrt(-jnp.expm1(2.0 * log_a)) * (i * y).astype(jnp.float32)
    return a, u


def linear_scan(a, u, reverse):
    def combine(lhs, rhs):
        a_l, u_l = lhs
        a_r, u_r = rhs
        return a_l * a_r, a_r * u_l + u_r
    return lax.associative_scan(combine, (a, u), reverse=reverse, axis=1)


def rglru_mixer(x_l, g_l, x_c, g_c, conv_w, conv_b, wa, ba, wi, bi, lam, need_ctx):
    y_l = short_conv(x_l, conv_w, conv_b)
    y_c = short_conv(x_c, conv_w, conv_b)
    h_l, h_c = [], []
    for d, rev in enumerate((False, True)):
        a_c, u_c = lru_coeffs(y_c, wa[d], ba[d], wi[d], bi[d], lam[d])
        _, s_c = linear_scan(a_c, u_c, rev)
        h0 = s_c[:, 0] if rev else s_c[:, -1]
        a_l, u_l = lru_coeffs(y_l, wa[d], ba[d], wi[d], bi[d], lam[d])
        cum_a, s_l = linear_scan(a_l, u_l, rev)
        h_l.append(cum_a * h0[:, None, :] + s_l)
        h_c.append(s_c)
    out_l = (h_l[0] + h_l[1]).astype(x_l.dtype) * jax.nn.gelu(g_l)
    out_c = (h_c[0] + h_c[1]).astype(x_c.dtype) * jax.nn.gelu(g_c) if need_ctx else None
    return out_l, out_c


def diff_core(q1, q2, k1, k2, v, lam):
    scale = DA_QK ** -0.5
    p1 = jax.nn.softmax(jnp.einsum('bhqd,bhkd->bhqk', q1, k1).astype(jnp.float32) * scale, axis=-1)
    p2 = jax.nn.softmax(jnp.einsum('bhqd,bhkd->bhqk', q2, k2).astype(jnp.float32) * scale, axis=-1)
    return jnp.einsum('bhqk,bhkd->bhqd', (p1 - lam * p2).astype(v.dtype), v)


def diff_attention(q_l, k_l, v_l, q_c, k_c, v_c, lam_vecs, norm_g, lam_init, cos, sin, need_ctx):
    lv = lam_vecs.astype(jnp.float32)
    lam = jnp.exp(jnp.sum(lv[0] * lv[1])) - jnp.exp(jnp.sum(lv[2] * lv[3])) + lam_init

    def split_qk(t, rotate):
        bsz, n, _ = t.shape
        t = t.reshape(bsz, n, DA_HEADS, 2, DA_QK)
        t1, t2 = t[..., 0, :], t[..., 1, :]
        if rotate:
            t1 = apply_rotary(t1, cos[:, None], sin[:, None])
            t2 = apply_rotary(t2, cos[:, None], sin[:, None])
        return t1.transpose(0, 2, 1, 3), t2.transpose(0, 2, 1, 3)

    def split_v(t):
        bsz, n, _ = t.shape
        return t.reshape(bsz, n, DA_HEADS, DA_V).transpose(0, 2, 1, 3)

    def finish(o):
        bsz, _, n, _ = o.shape
        o = rms_norm(o, norm_g) * (1.0 - lam_init)
        return o.transpose(0, 2, 1, 3).reshape(bsz, n, DA_WIDTH)

    q1c, q2c = split_qk(q_c, False)
    k1c, k2c = split_qk(k_c, False)
    vc = split_v(v_c)
    q1l, q2l = split_qk(q_l, True)
    k1l, k2l = split_qk(k_l, True)
    vl = split_v(v_l)
    k1 = jnp.concatenate([k1c, k1l], axis=2)
    k2 = jnp.concatenate([k2c, k2l], axis=2)
    v = jnp.concatenate([vc, vl], axis=2)
    o_l = sweep_query_blocks(lambda a, b: diff_core(a, b, k1, k2, v, lam), q1l, q2l)
    o_c = finish(diff_core(q1c, q2c, k1c, k2c, vc, lam)) if need_ctx else None
    return finish(o_l), o_c


def mla_core(q_nope, q_rope, k_nope, k_rope, v):
    s = jnp.einsum('bhqd,bhkd->bhqk', q_nope, k_nope) + jnp.einsum('bhqr,bkr->bhqk', q_rope, k_rope)
    p = jax.nn.softmax(s.astype(jnp.float32) * MLA_SCALE, axis=-1)
    return jnp.einsum('bhqk,bhkd->bhqd', p.astype(v.dtype), v)


def mla_attention(cq_l, ckv_l, kr_l, cq_c, ckv_c, kr_c, q_norm, kv_norm, w_uq, w_ukv, cos, sin, need_ctx):
    def project(cq, ckv):
        bsz, n, _ = cq.shape
        q = (rms_norm(cq, q_norm) @ w_uq).reshape(bsz, n, MLA_HEADS, MLA_NOPE + MLA_ROPE)
        kv = (rms_norm(ckv, kv_norm) @ w_ukv).reshape(bsz, n, MLA_HEADS, MLA_NOPE + MLA_V)
        return q[..., :MLA_NOPE], q[..., MLA_NOPE:], kv[..., :MLA_NOPE], kv[..., MLA_NOPE:]

    def heads_first(t):
        return t.transpose(0, 2, 1, 3)

    bsz, n, _ = cq_l.shape
    qn_l, qr_l, kn_l, v_l = project(cq_l, ckv_l)
    qr_l = apply_rotary(qr_l, cos[:, None], sin[:, None])
    kr_l = apply_rotary(kr_l, cos, sin)
    qn_c, qr_c, kn_c, v_c = project(cq_c, ckv_c)
    kn_all = jnp.concatenate([heads_first(kn_c), heads_first(kn_l)], axis=2)
    v_all = jnp.concatenate([heads_first(v_c), heads_first(v_l)], axis=2)
    kr_all = jnp.concatenate([kr_c, kr_l], axis=1)
    o_l = sweep_query_blocks(lambda a, b: mla_core(a, b, kn_all, kr_all, v_all), heads_first(qn_l), heads_first(qr_l))
    out_l = o_l.transpose(0, 2, 1, 3).reshape(bsz, n, MLA_WIDTH)
    out_c = None
    if need_ctx:
        o_c = mla_core(heads_first(qn_c), heads_first(qr_c), heads_first(kn_c), kr_c, heads_first(v_c))
        out_c = o_c.transpose(0, 2, 1, 3).reshape(bsz, cq_c.shape[1], MLA_WIDTH)
    return out_l, out_c


def moe_ffn(t, router_w, router_b, w1, w3, w2):
    scores = jax.nn.sigmoid((t @ router_w).astype(jnp.float32))
    sel = scores + router_b.astype(jnp.float32)
    grp_score = jnp.sum(lax.top_k(sel.reshape(-1, N_GROUPS, EXPERTS_PER_GROUP), TOP_K)[0], axis=-1)
    best = jnp.argmax(grp_score, axis=-1)
    in_group = (jnp.arange(N_EXPERTS) // EXPERTS_PER_GROUP)[None, :] == best[:, None]
    _, idx = lax.top_k(jnp.where(in_group, sel, -jnp.inf), TOP_K)
    w = jnp.take_along_axis(scores, idx, axis=-1)
    w = w / jnp.sum(w, axis=-1, keepdims=True)
    gates = jnp.sum(jax.nn.one_hot(idx, N_EXPERTS, dtype=jnp.float32) * w[..., None], axis=1).astype(t.dtype)
    y = jnp.zeros_like(t)
    for e in range(N_EXPERTS):
        h = jax.nn.silu(t @ w1[e]) * (t @ w3[e])
        y = y + gates[:, e:e + 1] * (h @ w2[e])
    return y


def setup_inputs(seed: int = 0) -> dict:
    key = jax.random.key(seed)
    ks = jax.random.split(key, 32)

    def nrm(i, shape, scale):
        return jax.random.normal(ks[i], shape, jnp.float32) * scale

    def gain(i, shape):
        return 1.0 + nrm(i, shape, 0.02)

    u = jax.random.uniform(ks[14], (DEPTH, 2, LRU_WIDTH), jnp.float32, 0.9, 0.999)
    s = u ** (1.0 / LRU_C)
    lru_lambda = jnp.log(s) - jnp.log1p(-s)
    return {
        "x": nrm(0, (BATCH, SEQ, D_MODEL), 1.0),
        "c": nrm(1, (BATCH, D_MODEL), 1.0),
        "ctx": nrm(2, (BATCH, CTX_LEN, D_MODEL), 1.0),
        "c_ctx": nrm(3, (D_MODEL,), 1.0),
        "w_mod": nrm(4, (DEPTH, D_MODEL, 6 * D_MODEL), 0.5 * D_MODEL ** -0.5),
        "b_mod": nrm(5, (DEPTH, 6 * D_MODEL), 0.02),
        "w_in": nrm(6, (DEPTH, D_MODEL, D_IN), D_MODEL ** -0.5),
        "w_out": nrm(7, (DEPTH, D_MIX, D_MODEL), DEEPNORM_BETA * D_MIX ** -0.5),
        "conv_w": nrm(8, (DEPTH, CONV_W, LRU_WIDTH), CONV_W ** -0.5),
        "conv_b": nrm(9, (DEPTH, LRU_WIDTH), 0.02),
        "lru_wa": nrm(10, (DEPTH, 2, LRU_BLOCKS, LRU_BLOCK, LRU_BLOCK), LRU_BLOCK ** -0.5),
        "lru_ba": nrm(11, (DEPTH, 2, LRU_WIDTH), 0.02),
        "lru_wi": nrm(12, (DEPTH, 2, LRU_BLOCKS, LRU_BLOCK, LRU_BLOCK), LRU_BLOCK ** -0.5),
        "lru_bi": nrm(13, (DEPTH, 2, LRU_WIDTH), 0.02),
        "lru_lambda": lru_lambda,
        "diff_lambda": nrm(15, (DEPTH, 4, DA_QK), 0.1),
        "diff_norm": gain(16, (DEPTH, DA_V)),
        "mla_q_norm": gain(17, (DEPTH, Q_RANK)),
        "mla_kv_norm": gain(18, (DEPTH, KV_RANK)),
        "mla_w_uq": nrm(19, (DEPTH, Q_RANK, MLA_HEADS * (MLA_NOPE + MLA_ROPE)), Q_RANK ** -0.5),
        "mla_w_ukv": nrm(20, (DEPTH, KV_RANK, MLA_HEADS * (MLA_NOPE + MLA_V)), KV_RANK ** -0.5),
        "ln1_g": gain(21, (DEPTH, D_MODEL)),
        "ln1_b": nrm(22, (DEPTH, D_MODEL), 0.02),
        "ln2_g": gain(23, (DEPTH, D_MODEL)),
        "ln2_b": nrm(24, (DEPTH, D_MODEL), 0.02),
        "router_w": nrm(25, (D_MODEL, N_EXPERTS), D_MODEL ** -0.5),
        "router_b": nrm(26, (N_EXPERTS,), 0.01),
        "exp_w1": nrm(27, (DEPTH, N_EXPERTS, D_MODEL, D_EXPERT), D_MODEL ** -0.5),
        "exp_w3": nrm(28, (DEPTH, N_EXPERTS, D_MODEL, D_EXPERT), D_MODEL ** -0.5),
        "exp_w2": nrm(29, (DEPTH, N_EXPERTS, D_EXPERT, D_MODEL), DEEPNORM_BETA * D_EXPERT ** -0.5),
    }


def reference(x, c, ctx, c_ctx, w_mod, b_mod, w_in, w_out, conv_w, conv_b,
              lru_wa, lru_ba, lru_wi, lru_bi, lru_lambda, diff_lambda, diff_norm,
              mla_q_norm, mla_kv_norm, mla_w_uq, mla_w_ukv,
              ln1_g, ln1_b, ln2_g, ln2_b, router_w, router_b, exp_w1, exp_w3, exp_w2):
    bsz, n, d = x.shape
    ctx_len = ctx.shape[1]
    cos_da, sin_da = axial_rotary(n, DA_QK)
    cos_mla, sin_mla = axial_rotary(n, MLA_ROPE)
    s_lat = jax.nn.silu(c)
    s_ctx = jax.nn.silu(c_ctx)
    x_l, x_c = x, ctx
    for l in range(DEPTH):
        need_ctx = l < DEPTH - 1
        mod_l = jnp.split((s_lat @ w_mod[l] + b_mod[l])[:, None, :], 6, axis=-1)
        mod_c = jnp.split(s_ctx @ w_mod[l] + b_mod[l], 6, axis=-1)
        u_l = x_l * (1.0 + mod_l[1]) + mod_l[0]
        u_c = x_c * (1.0 + mod_c[1]) + mod_c[0]
        p_l = split_in(u_l @ w_in[l])
        p_c = split_in(u_c @ w_in[l])
        a_l, a_c = rglru_mixer(p_l[0], p_l[1], p_c[0], p_c[1], conv_w[l], conv_b[l],
                               lru_wa[l], lru_ba[l], lru_wi[l], lru_bi[l], lru_lambda[l], need_ctx)
        lam_init = 0.8 - 0.6 * math.exp(-0.3 * l)
        b_l, b_c = diff_attention(p_l[2], p_l[3], p_l[4], p_c[2], p_c[3], p_c[4],
                                  diff_lambda[l], diff_norm[l], lam_init, cos_da, sin_da, need_ctx)
        m_l, m_c = mla_attention(p_l[5], p_l[6], p_l[7], p_c[5], p_c[6], p_c[7],
                                 mla_q_norm[l], mla_kv_norm[l], mla_w_uq[l], mla_w_ukv[l],
                                 cos_mla, sin_mla, need_ctx)
        o_l = jnp.concatenate([a_l, b_l, m_l], axis=-1) @ w_out[l]
        x_l = layer_norm(DEEPNORM_ALPHA * x_l + mod_l[2] * o_l, ln1_g[l], ln1_b[l])
        v_l = x_l * (1.0 + mod_l[4]) + mod_l[3]
        if need_ctx:
            o_c = jnp.concatenate([a_c, b_c, m_c], axis=-1) @ w_out[l]
            x_c = layer_norm(DEEPNORM_ALPHA * x_c + mod_c[2] * o_c, ln1_g[l], ln1_b[l])
            v_c = x_c * (1.0 + mod_c[4]) + mod_c[3]
            f = moe_ffn(jnp.concatenate([v_l.reshape(-1, d), v_c.reshape(-1, d)], axis=0),
                        router_w, router_b, exp_w1[l], exp_w3[l], exp_w2[l])
            f_l = f[: bsz * n].reshape(bsz, n, d)
            f_c = f[bsz * n:].reshape(bsz, ctx_len, d)
            x_c = layer_norm(DEEPNORM_ALPHA * x_c + mod_c[5] * f_c, ln2_g[l], ln2_b[l])
        else:
            f_l = moe_ffn(v_l.reshape(-1, d), router_w, router_b, exp_w1[l], exp_w3[l], exp_w2[l]).reshape(bsz, n, d)
        x_l = layer_norm(DEEPNORM_ALPHA * x_l + mod_l[5] * f_l, ln2_g[l], ln2_b[l])
    return x_l
```

```python
import math
from contextlib import ExitStack

import numpy as np
import concourse.bass as bass
import concourse.mybir as mybir
from concourse.bass_utils import run_bass_kernel_spmd

F32 = mybir.dt.float32
BF16 = mybir.dt.bfloat16
AF = mybir.ActivationFunctionType
ALU = mybir.AluOpType
AX = mybir.AxisListType

D = 1024
KC = 8
DEPTH = 4
CTX = 256
GRID_W = 64
NE = 16
DE = 256
ALPHA = (2 * DEPTH) ** 0.25
LN_EPS_EFF = 1e-5 / (ALPHA * ALPHA)
RMS_EPS = 1e-6
DA_SCALE = 32 ** -0.5
MLA_SCALE = 96 ** -0.5
WIN_COLS = 2880
C_XB, C_GATE, C_QDA, C_KDA, C_VDA, C_CQ, C_CKV, C_KR = 0, 256, 512, 896, 1280, 1664, 1920, 2048
C_QDA_SW, C_KDA_SW, C_KR_SW = 2080, 2464, 2848


class S:
    EPOCH = 16000
    NDMA = 20

    def __init__(self, nc, es):
        self.nc = nc
        self.es = es
        self.eng = {"pe": nc.tensor, "act": nc.scalar, "dve": nc.vector, "pool": nc.gpsimd, "sp": nc.sync}
        self.names = list(self.eng)
        self.esems = {e: [] for e in self.names}
        self.count = {e: 0 for e in self.names}
        self.waited = {e: {e2: 0 for e2 in self.names} for e in self.names}
        self.pe_pending = False
        self.dma_sems = {}
        self.dma_rr = {}
        self.dma_total = {}
        self.dma_waited = {e: {} for e in self.names}
        for q in ("sp", "act", "pool"):
            self.dma_sems[q] = [es.enter_context(nc.semaphore(f"dq_{q}_{i}")) for i in range(self.NDMA)]
            self.dma_rr[q] = 0
            for s_ in self.dma_sems[q]:
                self.dma_total[s_.name] = 0
        self.sem_by_name = {s_.name: s_ for q in self.dma_sems for s_ in self.dma_sems[q]}
        self.bar_sem = es.enter_context(nc.semaphore("barrier"))
        self.bar_count = 0
        self.last_w = {}
        self.readers = {}
        self.last_x = {}
        self.n_ops = 0

    def _esem(self, e, n):
        k = (n - 1) // self.EPOCH
        while len(self.esems[e]) <= k:
            self.esems[e].append(self.es.enter_context(self.nc.semaphore(f"es_{e}_{len(self.esems[e])}")))
        return self.esems[e][k], n - k * self.EPOCH

    def _wait(self, e, tok):
        if tok[0] == "e":
            _, e2, n = tok
            if self.waited[e][e2] >= n:
                return
            assert n <= self.count[e2], f"waiting on unsignalled op of {e2}: {n} > {self.count[e2]}"
            sem, val = self._esem(e2, n)
            self.eng[e].wait_ge(sem, val)
            self.waited[e][e2] = n
        else:
            _, sname, val = tok
            if self.dma_waited[e].get(sname, 0) >= val:
                return
            self.eng[e].wait_ge(self.sem_by_name[sname], val)
            self.dma_waited[e][sname] = val

    def _deps(self, e, reads, writes, psum):
        raw = set()
        oth = set()
        for k in reads:
            t = self.last_w.get(k)
            if t is not None:
                raw.add(t)
        for k in writes:
            t = self.last_w.get(k)
            if t is not None:
                oth.add(t)
            for t in self.readers.get(k, ()):
                oth.add(t)
        for k in psum:
            t = self.last_x.get(k)
            if t is not None:
                oth.add(t)
        for t in raw:
            if t[0] == "e" and t[1] == e and e == "pe":
                continue
            self._wait(e, t)
        for t in oth:
            if t[0] == "e" and t[1] == e:
                continue
            self._wait(e, t)

    def _register(self, tok, reads, writes, psum):
        for k in reads:
            self.readers.setdefault(k, []).append(tok)
        for k in writes:
            self.last_w[k] = tok
            self.readers[k] = []
        for k in psum:
            self.last_x[k] = tok

    def op(self, e, fn, reads=(), writes=(), psum=(), signal=True):
        self._deps(e, reads, writes, psum)
        ins = fn()
        self.n_ops += 1
        if signal:
            self.count[e] += 1
            sem, _ = self._esem(e, self.count[e])
            ins.then_inc(sem, 1)
            tok = ("e", e, self.count[e])
            if e == "pe":
                self.pe_pending = False
        else:
            assert e == "pe"
            tok = ("e", e, self.count[e] + 1)
            self.pe_pending = True
        self._register(tok, reads, writes, psum)
        return tok

    def dma(self, q, out, in_, reads=(), writes=(), after=(), **kw):
        self._deps(q, reads, writes, ())
        for t in after:
            self._wait(q, t)
        i = self.dma_rr[q]
        self.dma_rr[q] = (i + 1) % self.NDMA
        sem = self.dma_sems[q][i]
        tot = self.dma_total[sem.name]
        if tot > 0:
            self._wait(q, ("d", sem.name, tot))
        self.eng[q].dma_start(out=out, in_=in_, **kw).then_inc(sem, 16)
        self.n_ops += 1
        self.dma_total[sem.name] = tot + 16
        tok = ("d", sem.name, tot + 16)
        self._register(tok, reads, writes, ())
        return tok

    def barrier(self):
        assert not self.pe_pending
        for e2 in self.names:
            if e2 != "sp" and self.count[e2] > 0:
                self._wait("sp", ("e", e2, self.count[e2]))
        for name, tot in self.dma_total.items():
            if tot > 0 and not name.startswith("dq_pool"):
                self._wait("sp", ("d", name, tot))
        self.bar_count += 1
        self.eng["sp"].sem_inc(self.bar_sem, 1)
        for e in self.names:
            if e != "sp":
                self.eng[e].wait_ge(self.bar_sem, self.bar_count)
        for e in self.names:
            for e2 in self.names:
                self.waited[e][e2] = self.count[e2]
            for name, tot in self.dma_total.items():
                if not name.startswith("dq_pool"):
                    self.dma_waited[e][name] = tot
        self.last_w.clear()
        self.readers.clear()
        self.last_x.clear()

    def finish(self):
        assert not self.pe_pending
        for e2 in self.names:
            if e2 != "sp" and self.count[e2] > 0:
                self._wait("sp", ("e", e2, self.count[e2]))
        for name, tot in self.dma_total.items():
            if tot > 0:
                self._wait("sp", ("d", name, tot))


class Banks:
    def __init__(self, nc, es):
        self.t = [es.enter_context(nc.psum_tensor(f"psum{i}", [128, 2, 512], F32)) for i in range(4)]
        self.rr = 0

    def bank(self, i):
        return self.t[i // 2][:, i % 2, :]

    def pair(self, i):
        return self.t[i]

    def next(self):
        i = self.rr
        self.rr = (self.rr + 1) % 8
        return i


def bkey(i):
    return ("psb", i)


def build_program(lat=4096, n_layers=DEPTH, nb=2, debug=(), stop_after=None):
    ntok = CTX + lat
    nkb = ntok // 128
    chunks = [(0, CTX, True)] + [(CTX + 512 * i, 512, False) for i in range(lat // 512)]
    XBW = ntok + 6
    nc = bass.Bass("TRN2", target_bir_lowering=False)

    def din(name, shape, dt=F32):
        return nc.dram_tensor(name, list(shape), dt, kind="ExternalInput").ap()

    def dscr(name, shape, dt):
        kind = "ExternalOutput" if name in debug else "Internal"
        return nc.dram_tensor(name, list(shape), dt, kind=kind).ap()

    x_in = din("x", [nb, lat, D])
    ctx_in = din("ctx", [nb, CTX, D])
    cT_in = din("cT", [128, KC, 4])
    wmod_in = din("w_mod_r", [n_layers, 12, 128, KC, 512])
    bmod_in = din("b_mod_r", [128, n_layers, 48])
    win_in = din("w_in_r", [n_layers, 128, KC, WIN_COLS])
    wout_in = din("w_out_r", [n_layers, 128, KC, D])
    wuq_in = din("w_uq_r", [n_layers, 128, 2, 768])
    wukv_in = din("w_ukv_r", [n_layers, 128, 768])
    lruw_in = din("lru_w_r", [n_layers, 128, 8, 128])
    w13_in = din("w13_r", [n_layers, NE, 128, 2, KC, DE])
    w2_in = din("w2_r", [n_layers, 8, 128, NE, 2, 128])
    router_in = din("router_r", [128, KC, NE])
    rb_in = din("router_b_r", [128, NE])
    small_in = din("small_r", [128, n_layers, 40])
    dlam_in = din("dlam_r", [128, n_layers, 4, 32])
    cos_in = din("cosT", [128, lat])
    sin_in = din("sinT", [128, lat])
    ident_in = din("ident", [128, 128])
    selm_in = din("selm", [16, NE, 128], BF16)
    out_d = nc.dram_tensor("out", [nb, lat, D], F32, kind="ExternalOutput").ap()

    win_b = dscr("win_b", [n_layers, 128, KC, WIN_COLS], BF16)
    wout_b = dscr("wout_b", [n_layers, 128, KC, D], BF16)
    wuq_b = dscr("wuq_b", [n_layers, 128, 2, 768], BF16)
    wukv_b = dscr("wukv_b", [n_layers, 128, 768], BF16)
    lruw_b = dscr("lruw_b", [n_layers, 128, 8, 128], BF16)
    w13_b = dscr("w13_b", [n_layers, NE, 128, 2, KC, DE], BF16)
    w2_b = dscr("w2_b", [n_layers, 8, 128, NE, 2, 128], BF16)
    xT_s = dscr("xT_s", [D, ntok], F32)
    xb_s = dscr("xb_s", [256, XBW], F32)
    gg_s = dscr("gg_s", [256, ntok], F32)
    hf_s = dscr("hf_s", [256, ntok], F32)
    hb_s = dscr("hb_s", [256, ntok], F32)
    aT_s = dscr("aT_s", [256, ntok], BF16)
    qda_s = dscr("qda_s", [384, ntok], BF16)
    kda_s = dscr("kda_s", [384, ntok], BF16)
    vda_s = dscr("vda_s", [ntok, 576], BF16)
    qn_s = dscr("qn_s", [384, ntok], BF16)
    qr_s = dscr("qr_s", [192, ntok], BF16)
    kn_s = dscr("kn_s", [384, ntok], BF16)
    kr_s = dscr("kr_s", [32, ntok], BF16)
    vml_s = dscr("vml_s", [ntok, 576], BF16)
    bT_s = dscr("bT_s", [384, ntok], BF16)
    mT_s = dscr("mT_s", [384, ntok], BF16)

    with ExitStack() as es:
        s = S(nc, es)
        pb = Banks(nc, es)

        uniq = [0]

        def sb(stack, name, shape, dt=F32):
            uniq[0] += 1
            return stack.enter_context(nc.sbuf_tensor(f"{name}_{uniq[0]}", list(shape), dt))

        mod = sb(es, "mod", [128, n_layers, 48, 4])
        small = sb(es, "small", [128, n_layers, 40])
        coef = sb(es, "coef", [128, n_layers, 8])
        negl = sb(es, "negl", [128, n_layers])
        gn = sb(es, "gn", [128, n_layers])
        ones_b = sb(es, "ones_b", [128, 128], BF16)
        ones_f = sb(es, "ones_f", [128, 128], F32)
        ident = sb(es, "ident", [128, 128], F32)
        router = sb(es, "router", [128, KC, NE], F32)
        rbias = sb(es, "rbias", [128, NE], F32)
        small2 = sb(es, "small2", [128, n_layers, 24])
        small2_in = din("small2_r", [128, n_layers, 16])
        lam_in = din("lam_r", [128, n_layers, 4])

        conv_tok = {}

        def convert_layer(l_):
            for (dst, src, lead) in ((win_b, win_in, ()), (wout_b, wout_in, ()), (wuq_b, wuq_in, ()), (wukv_b, wukv_in, ()),
                                     (lruw_b, lruw_in, ()), (w13_b, w13_in, (NE,)), (w2_b, w2_in, (8,))):
                toks = []
                for idx in np.ndindex(*lead):
                    full = (l_,) + idx
                    toks.append(s.dma("pool", dst[full], src[full], max_dma_last_dim=4096))
                conv_tok[(dst.name, l_)] = toks

        def wtok(ap_, l_):
            return conv_tok[(ap_.name, l_)]

        with ExitStack() as ps:
            cT = sb(ps, "cT", [128, KC, 4])
            sT = sb(ps, "sT", [128, KC, 4])
            bmod = sb(ps, "bmod", [128, n_layers, 48])
            dlam = sb(ps, "dlam", [128, n_layers, 4, 32])
            dl_p = sb(ps, "dl_p", [128, n_layers, 2, 32])
            dl_s = sb(ps, "dl_s", [128, n_layers, 2])
            dl_e = sb(ps, "dl_e", [128, n_layers, 2])
            lam_t = sb(ps, "lam_t", [128, n_layers, 4])
            lam_e = sb(ps, "lam_e", [128, n_layers, 4])
            zer = sb(ps, "zer", [128, 2, 4])
            wm = [sb(ps, f"wm{i}", [128, KC, 512]) for i in range(2)]

            s.dma("sp", cT[:], cT_in[:, :, :], writes=["cT"])
            s.dma("sp", bmod[:], bmod_in[:, :, :], writes=["bmod"])
            s.dma("sp", small[:], small_in[:, :, :], writes=["small"])
            s.dma("sp", small2[:, :, 0:16], small2_in[:, :, :], writes=["small2"])
            s.op("dve", lambda: nc.vector.tensor_scalar(out=small2[:, :, 16:24], in0=small2[:, :, 8:16], scalar1=-1.0, scalar2=None, op0=ALU.mult),
                 reads=["small2"], writes=["small2"])
            s.dma("sp", dlam[:], dlam_in[:, :, :, :], writes=["dlam"])
            s.dma("sp", lam_t[:], lam_in[:, :, :], writes=["lam_t"])
            s.dma("sp", ident[:], ident_in[:, :], writes=["ident"])
            s.dma("sp", router[:], router_in[:, :, :], writes=["router"])
            s.dma("sp", rbias[:], rb_in[:, :], writes=["rbias"])
            s.op("pool", lambda: nc.gpsimd.memset(ones_b[:], 1.0), writes=["ones_b"])
            s.op("pool", lambda: nc.gpsimd.memset(ones_f[:], 1.0 / D), writes=["ones_f"])
            s.op("pool", lambda: nc.gpsimd.memset(zer[:], 0.0), writes=["zer"])
            for (a, b_) in ((0, 2), (258, 261), (XBW - 1, XBW)):
                s.dma("sp", xb_s.rearrange("(c p) t -> p c t", p=128)[:, :, a:b_], zer[:, :, 0:b_ - a],
                      reads=["zer"], writes=[("xb_pad", a)], allow_slow_non_contiguous=True)

            convert_layer(0)

            s.op("act", lambda: nc.scalar.activation(out=sT[:], in_=cT[:], func=AF.Silu), reads=["cT"], writes=["sT"])
            for l in range(n_layers):
                bk = pb.next()
                bank = pb.bank(bk)
                for g in range(12):
                    w = wm[g % 2]
                    wk = ("wm", g % 2)
                    s.dma("sp", w[:], wmod_in[l, g], writes=[wk])
                    for jj in range(4):
                        j = g * 4 + jj
                        for kc in range(KC):
                            s.op("pe", lambda: nc.tensor.matmul(bank[:, j * 4:j * 4 + 4], w[:, kc, jj * 128:(jj + 1) * 128],
                                                                sT[:, kc, :], start=(kc == 0), stop=(kc == KC - 1)),
                                 reads=[wk, "sT"], writes=[bkey(bk)], psum=[bkey(bk)], signal=(kc == KC - 1))
                s.op("dve", lambda: nc.vector.tensor_tensor(
                    out=mod[:, l, :, :], in0=bank[:, 0:192].rearrange("p (j b) -> p j b", b=4),
                    in1=bmod[:, l, :].unsqueeze(2).to_broadcast([128, 48, 4]), op=ALU.add),
                    reads=[bkey(bk), "bmod"], writes=["mod"], psum=[bkey(bk)])
                for g_ in (1, 4):
                    s.op("dve", lambda: nc.vector.tensor_scalar(out=mod[:, l, g_ * 8:g_ * 8 + 8, :], in0=mod[:, l, g_ * 8:g_ * 8 + 8, :],
                                                                 scalar1=1.0, scalar2=None, op0=ALU.add),
                         reads=["mod"], writes=["mod"])
                for g_ in (2, 5):
                    s.op("dve", lambda: nc.vector.tensor_scalar(out=mod[:, l, g_ * 8:g_ * 8 + 8, :], in0=mod[:, l, g_ * 8:g_ * 8 + 8, :],
                                                                 scalar1=1.0 / ALPHA, scalar2=None, op0=ALU.mult),
                         reads=["mod"], writes=["mod"])
            s.op("dve", lambda: nc.vector.tensor_tensor(out=dl_p[:], in0=dlam[:, :, 0:4:2, :], in1=dlam[:, :, 1:4:2, :], op=ALU.mult),
                 reads=["dlam"], writes=["dl_p"])
            s.op("dve", lambda: nc.vector.tensor_reduce(out=dl_s[:], in_=dl_p[:], axis=AX.X, op=ALU.add),
                 reads=["dl_p"], writes=["dl_s"])
            s.op("act", lambda: nc.scalar.activation(out=dl_e[:], in_=dl_s[:], func=AF.Exp), reads=["dl_s"], writes=["dl_e"])
            s.op("dve", lambda: nc.vector.tensor_tensor(out=negl[:], in0=dl_e[:, :, 1], in1=dl_e[:, :, 0], op=ALU.subtract),
                 reads=["dl_e"], writes=["negl"])
            for l in range(n_layers):
                lam_init = 0.8 - 0.6 * math.exp(-0.3 * l)
                s.op("dve", lambda: nc.vector.tensor_scalar(out=negl[:, l:l + 1], in0=negl[:, l:l + 1], scalar1=-lam_init, scalar2=None, op0=ALU.add),
                     reads=["negl"], writes=["negl"])
                s.op("dve", lambda: nc.vector.tensor_scalar(out=gn[:, l:l + 1], in0=small[:, l, 37:38], scalar1=1.0 - lam_init, scalar2=None, op0=ALU.mult),
                     reads=["small"], writes=["gn"])
            s.op("act", lambda: nc.scalar.activation(out=lam_e[:], in_=lam_t[:], func=AF.Exp, scale=-1.0), reads=["lam_t"], writes=["lam_e"])
            s.op("act", lambda: nc.scalar.activation(out=lam_e[:], in_=lam_e[:], func=AF.Ln, bias=1.0), reads=["lam_e"], writes=["lam_e"])
            s.op("dve", lambda: nc.vector.tensor_scalar(out=coef[:, :, 0:4], in0=lam_e[:], scalar1=-8.0, scalar2=None, op0=ALU.mult),
                 reads=["lam_e"], writes=["coef"])
            s.op("dve", lambda: nc.vector.tensor_scalar(out=coef[:, :, 4:8], in0=lam_e[:], scalar1=-16.0, scalar2=None, op0=ALU.mult),
                 reads=["lam_e"], writes=["coef"])
            s.barrier()

        def fm(scr, nch):
            return scr.rearrange("(c p) t -> p c t", p=128)

        def modcol(l, g, kc, bi):
            return mod[:, l, g * 8 + kc, bi:bi + 1]

        for b in range(nb):
            with ExitStack() as ps:
                xtok = [sb(ps, f"xtok{i}", [128, 4, D]) for i in range(2)]
                xTc = [sb(ps, f"xTc{i}", [128, KC, 512]) for i in range(2)]
                for ci, (pos, N, isctx) in enumerate(chunks):
                    nt = N // 128
                    xt = xtok[ci % 2]
                    xk = ("xtok", ci % 2)
                    xc = xTc[ci % 2]
                    ck = ("xTc", ci % 2)
                    src = ctx_in[b] if isctx else x_in[b, pos - CTX:pos - CTX + N, :]
                    s.dma("sp", xt[:, 0:nt, :], src.rearrange("(t p) d -> p t d", p=128), writes=[xk])
                    for kc in range(KC):
                        bk = pb.next()
                        bank = pb.bank(bk)
                        for t in range(nt):
                            s.op("pe", lambda: nc.tensor.transpose(bank[:, t * 128:(t + 1) * 128], xt[:, t, kc * 128:(kc + 1) * 128], ident[:]),
                                 reads=[xk, "ident"], writes=[bkey(bk)], psum=[bkey(bk)], signal=(t == nt - 1))
                        eng = "act" if kc % 2 == 0 else "dve"
                        if eng == "act":
                            s.op("act", lambda: nc.scalar.copy(out=xc[:, kc, 0:N], in_=bank[:, 0:N]),
                                 reads=[bkey(bk)], writes=[ck], psum=[bkey(bk)])
                        else:
                            s.op("dve", lambda: nc.vector.tensor_copy(out=xc[:, kc, 0:N], in_=bank[:, 0:N]),
                                 reads=[bkey(bk)], writes=[ck], psum=[bkey(bk)])
                    s.dma("sp", fm(xT_s, KC)[:, :, pos:pos + N], xc[:, :, 0:N], reads=[ck], writes=[("xT", ci)])
                s.barrier()

            for l in range(n_layers):
                need_ctx = l < n_layers - 1
                lam_init = 0.8 - 0.6 * math.exp(-0.3 * l)
                conv_next = (lambda l_=l: convert_layer(l_ + 1)) if (b == 0 and l + 1 < n_layers) else None
                layer_phases(nc, s, pb, sb, locals())

        s.finish()
    return nc


def layer_phases(nc, s, pb, sb, env):
    g = env
    b, l, need_ctx = g["b"], g["l"], g["need_ctx"]
    n_layers, lat, ntok, nkb, chunks, XBW = g["n_layers"], g["lat"], g["ntok"], g["nkb"], g["chunks"], g["XBW"]
    mod, small, small2, coef, negl, gn = g["mod"], g["small"], g["small2"], g["coef"], g["negl"], g["gn"]
    ones_b, ones_f, ident, router, rbias = g["ones_b"], g["ones_f"], g["ident"], g["router"], g["rbias"]
    fm, modcol = g["fm"], g["modcol"]
    last = (l == n_layers - 1)

    def mb(isctx):
        return 2 if isctx else b

    with ExitStack() as ps:
        W = sb(ps, "W", [128, KC, WIN_COLS], BF16)
        Wuq = sb(ps, "Wuq", [128, 2, 768], BF16)
        Wukv = sb(ps, "Wukv", [128, 768], BF16)
        xT = [sb(ps, f"xT{i}", [128, KC, 512]) for i in range(2)]
        U = sb(ps, "U", [128, KC, 512], BF16)
        cosT = sb(ps, "cosT_t", [128, 512])
        sinT = sb(ps, "sinT_t", [128, 512])
        tA = [sb(ps, f"tA{i}", [128, 512]) for i in range(2)]
        tB = [sb(ps, f"tB{i}", [128, 512]) for i in range(2)]
        of32 = [sb(ps, f"of32_{i}", [128, 512]) for i in range(4)]
        obf = [sb(ps, f"obf_{i}", [128, 512], BF16) for i in range(4)]
        sq = sb(ps, "sq", [128, 3, 512], BF16)
        rstd = sb(ps, "rstd", [128, 2, 512])
        cqn = sb(ps, "cqn", [128, 2, 512], BF16)
        ckvn = sb(ps, "ckvn", [128, 512], BF16)
        vtok = [sb(ps, f"vtok{i}", [128, 576], BF16) for i in range(2)]
        cnt = {"t": 0, "o": 0, "b": 0, "v": 0}

        s.dma("sp", W[:], g["win_b"][l], writes=["W"], after=g["wtok"](g["win_b"], l))
        s.dma("sp", Wuq[:], g["wuq_b"][l], writes=["Wuq"], after=g["wtok"](g["wuq_b"], l))
        s.dma("sp", Wukv[:], g["wukv_b"][l], writes=["Wukv"], after=g["wtok"](g["wukv_b"], l))
        for i in range(2):
            for c in range(3):
                s.op("pool", lambda: nc.gpsimd.memset(vtok[i][:, c * 192 + 64:c * 192 + 128], 1.0), writes=[("vtok", i)])

        def next_obf():
            i = cnt["b"] % 4
            cnt["b"] += 1
            return obf[i], ("obf", i)

        def next_of32():
            i = cnt["o"] % 4
            cnt["o"] += 1
            return of32[i], ("of32", i)

        def proj(col0, M, N, rhs=None, rk="U", lhs=None, lk="W", nk=KC, prow=0):
            bk = pb.next()
            bank = pb.bank(bk)
            lhs_ = W if lhs is None else lhs
            rhs_ = U if rhs is None else rhs
            for kc in range(nk):
                s.op("pe", lambda: nc.tensor.matmul(bank[prow:prow + M, 0:N], lhs_[:, kc, col0:col0 + M], rhs_[:, kc, 0:N],
                                                    start=(kc == 0), stop=(kc == nk - 1)),
                     reads=[lk, rk], writes=[bkey(bk)], psum=[bkey(bk)], signal=(kc == nk - 1))
            return bk

        def rot(bk_t, bk_sw, M, N, isctx, dst, dk):
            bt = pb.bank(bk_t)
            if isctx:
                s.op("act", lambda: nc.scalar.copy(out=dst[0:M, 0:N], in_=bt[0:M, 0:N]),
                     reads=[bkey(bk_t)], writes=[dk], psum=[bkey(bk_t)])
                return
            bs = pb.bank(bk_sw)
            i = cnt["t"] % 2
            cnt["t"] += 1
            s.op("dve", lambda: nc.vector.tensor_tensor(out=tA[i][0:M, 0:N], in0=bt[0:M, 0:N], in1=cosT[0:M, 0:N], op=ALU.mult),
                 reads=[bkey(bk_t), "cosT"], writes=[("tA", i)], psum=[bkey(bk_t)])
            s.op("dve", lambda: nc.vector.tensor_tensor(out=tB[i][0:M, 0:N], in0=bs[0:M, 0:N], in1=sinT[0:M, 0:N], op=ALU.mult),
                 reads=[bkey(bk_sw), "sinT"], writes=[("tB", i)], psum=[bkey(bk_sw)])
            s.op("pool", lambda: nc.gpsimd.tensor_tensor(out=dst[0:M, 0:N], in0=tA[i][0:M, 0:N], in1=tB[i][0:M, 0:N], op=ALU.add),
                 reads=[("tA", i), ("tB", i)], writes=[dk])

        for ci, (pos, N, isctx) in enumerate(chunks):
            bi = mb(isctx)
            x = xT[ci % 2]
            xk = ("xTl", ci % 2)
            s.dma("sp", x[:, :, 0:N], fm(g["xT_s"], KC)[:, :, pos:pos + N], reads=[("xT", ci)], writes=[xk])
            if not isctx:
                lp = pos - CTX
                s.dma("sp", cosT[:, 0:N], g["cos_in"][:, lp:lp + N], writes=["cosT"])
                s.dma("sp", sinT[:, 0:N], g["sin_in"][:, lp:lp + N], writes=["sinT"])
            for kc in range(KC):
                if kc % 2 == 0:
                    s.op("dve", lambda: nc.vector.tensor_scalar(out=U[:, kc, 0:N], in0=x[:, kc, 0:N], scalar1=modcol(l, 1, kc, bi),
                                                                 scalar2=modcol(l, 0, kc, bi), op0=ALU.mult, op1=ALU.add),
                         reads=[xk, "mod"], writes=["U"])
                else:
                    s.op("act", lambda: nc.scalar.activation(out=U[:, kc, 0:N], in_=x[:, kc, 0:N], func=AF.Identity,
                                                              scale=modcol(l, 1, kc, bi), bias=modcol(l, 0, kc, bi)),
                         reads=[xk, "mod"], writes=["U"])
            for c in range(2):
                bk = proj(C_XB + c * 128, 128, N)
                o, ok = next_of32()
                s.op("act", lambda: nc.scalar.copy(out=o[:, 0:N], in_=pb.bank(bk)[:, 0:N]), reads=[bkey(bk)], writes=[ok], psum=[bkey(bk)])
                off = pos + 2 if isctx else pos + 5
                s.dma("sp", g["xb_s"][c * 128:(c + 1) * 128, off:off + N], o[:, 0:N], reads=[ok], writes=[("xb", c, ci)])
                bk = proj(C_GATE + c * 128, 128, N)
                o, ok = next_of32()
                o2, ok2 = next_of32()
                s.op("act", lambda: nc.scalar.copy(out=o[:, 0:N], in_=pb.bank(bk)[:, 0:N]), reads=[bkey(bk)], writes=[ok], psum=[bkey(bk)])
                s.op("act", lambda: nc.scalar.activation(out=o2[:, 0:N], in_=pb.bank(bk)[:, 0:N], func=AF.Square), reads=[bkey(bk)], writes=[ok2], psum=[bkey(bk)])
                s.op("pool", lambda: nc.gpsimd.tensor_scalar(out=o2[:, 0:N], in0=o2[:, 0:N], scalar1=0.044715, scalar2=1.0, op0=ALU.mult, op1=ALU.add),
                     reads=[ok2], writes=[ok2])
                s.op("pool", lambda: nc.gpsimd.tensor_tensor(out=o2[:, 0:N], in0=o2[:, 0:N], in1=o[:, 0:N], op=ALU.mult), reads=[ok2, ok], writes=[ok2])
                s.op("act", lambda: nc.scalar.activation(out=o2[:, 0:N], in_=o2[:, 0:N], func=AF.Sigmoid, scale=2.0 * math.sqrt(2.0 / math.pi)),
                     reads=[ok2], writes=[ok2])
                s.op("pool", lambda: nc.gpsimd.tensor_tensor(out=o[:, 0:N], in0=o2[:, 0:N], in1=o[:, 0:N], op=ALU.mult), reads=[ok2, ok], writes=[ok])
                s.dma("sp", g["gg_s"][c * 128:(c + 1) * 128, pos:pos + N], o[:, 0:N], reads=[ok], writes=[("gg", c, ci)])
            for (c0, c0sw, scr, nm) in ((C_QDA, C_QDA_SW, g["qda_s"], "qda"), (C_KDA, C_KDA_SW, g["kda_s"], "kda")):
                for c in range(3):
                    bk_t = proj(c0 + c * 128, 128, N)
                    bk_s = None if isctx else proj(c0sw + c * 128, 128, N)
                    o, ok = next_obf()
                    rot(bk_t, bk_s, 128, N, isctx, o, ok)
                    s.dma("sp", scr[c * 128:(c + 1) * 128, pos:pos + N], o[:, 0:N], reads=[ok], writes=[(nm, c, ci)])
            bk_t = proj(C_KR, 32, N)
            bk_s = None if isctx else proj(C_KR_SW, 32, N)
            o, ok = next_obf()
            rot(bk_t, bk_s, 32, N, isctx, o, ok)
            s.dma("sp", g["kr_s"][:, pos:pos + N], o[0:32, 0:N], reads=[ok], writes=[("kr", ci)])
            for t in range(N // 128):
                bk = pb.next()
                bank = pb.bank(bk)
                for kc in range(KC):
                    s.op("pe", lambda: nc.tensor.matmul(bank[:, 0:384], U[:, kc, t * 128:(t + 1) * 128], W[:, kc, C_VDA:C_VDA + 384],
                                                        start=(kc == 0), stop=(kc == KC - 1)),
                         reads=["U", "W"], writes=[bkey(bk)], psum=[bkey(bk)], signal=(kc == KC - 1))
                vi = cnt["v"] % 2
                cnt["v"] += 1
                vt = vtok[vi]
                vv = vt[:, :].rearrange("p (c x) -> p c x", x=192)
                bv = bank[:, 0:384].rearrange("p (c x) -> p c x", x=128)
                s.op("act", lambda: nc.scalar.copy(out=vv[:, :, 0:64], in_=bv[:, :, 0:64]), reads=[bkey(bk)], writes=[("vtok", vi)], psum=[bkey(bk)])
                s.op("dve", lambda: nc.vector.tensor_copy(out=vv[:, :, 128:192], in_=bv[:, :, 64:128]), reads=[bkey(bk)], writes=[("vtok", vi)], psum=[bkey(bk)])
                s.dma("sp", g["vda_s"][pos + t * 128:pos + (t + 1) * 128, :], vt[:, :], reads=[("vtok", vi)], writes=[("vda", ci, t)])
            bk_cq = [proj(C_CQ + c * 128, 128, N) for c in range(2)]
            bk_ckv = proj(C_CKV, 128, N)
            for c in range(2):
                s.op("act", lambda: nc.scalar.activation(out=sq[:, c, 0:N], in_=pb.bank(bk_cq[c])[:, 0:N], func=AF.Square),
                     reads=[bkey(bk_cq[c])], writes=[("sq", c)], psum=[bkey(bk_cq[c])])
            s.op("act", lambda: nc.scalar.activation(out=sq[:, 2, 0:N], in_=pb.bank(bk_ckv)[:, 0:N], func=AF.Square),
                 reads=[bkey(bk_ckv)], writes=[("sq", 2)], psum=[bkey(bk_ckv)])
            bk_ss = pb.next()
            for c in range(2):
                s.op("pe", lambda: nc.tensor.matmul(pb.bank(bk_ss)[:, 0:N], ones_b[:, :], sq[:, c, 0:N], start=(c == 0), stop=(c == 1)),
                     reads=["ones_b", ("sq", c)], writes=[bkey(bk_ss)], psum=[bkey(bk_ss)], signal=(c == 1))
            bk_s2 = pb.next()
            s.op("pe", lambda: nc.tensor.matmul(pb.bank(bk_s2)[:, 0:N], ones_b[:, :], sq[:, 2, 0:N], start=True, stop=True),
                 reads=["ones_b", ("sq", 2)], writes=[bkey(bk_s2)], psum=[bkey(bk_s2)])
            for (bk_, j, dim) in ((bk_ss, 0, 256.0), (bk_s2, 1, 128.0)):
                s.op("act", lambda: nc.scalar.activation(out=rstd[:, j, 0:N], in_=pb.bank(bk_)[:, 0:N], func=AF.Ln, scale=1.0 / dim, bias=small[:, l, 38:39]),
                     reads=[bkey(bk_), "small"], writes=[("rstd", j)], psum=[bkey(bk_)])
                s.op("act", lambda: nc.scalar.activation(out=rstd[:, j, 0:N], in_=rstd[:, j, 0:N], func=AF.Exp, scale=-0.5),
                     reads=[("rstd", j)], writes=[("rstd", j)])
            for c in range(2):
                s.op("dve", lambda: nc.vector.scalar_tensor_tensor(out=cqn[:, c, 0:N], in0=pb.bank(bk_cq[c])[:, 0:N], scalar=small[:, l, 32 + c:33 + c],
                                                                    in1=rstd[:, 0, 0:N], op0=ALU.mult, op1=ALU.mult),
                     reads=[bkey(bk_cq[c]), "small", ("rstd", 0)], writes=["cqn"], psum=[bkey(bk_cq[c])])
            s.op("dve", lambda: nc.vector.scalar_tensor_tensor(out=ckvn[:, 0:N], in0=pb.bank(bk_ckv)[:, 0:N], scalar=small[:, l, 34:35],
                                                                in1=rstd[:, 1, 0:N], op0=ALU.mult, op1=ALU.mult),
                 reads=[bkey(bk_ckv), "small", ("rstd", 1)], writes=["ckvn"], psum=[bkey(bk_ckv)])
            for c in range(3):
                bk = proj(c * 128, 128, N, rhs=cqn, rk="cqn", lhs=Wuq, lk="Wuq", nk=2)
                o, ok = next_obf()
                s.op("act", lambda: nc.scalar.copy(out=o[:, 0:N], in_=pb.bank(bk)[:, 0:N]), reads=[bkey(bk)], writes=[ok], psum=[bkey(bk)])
                s.dma("sp", g["qn_s"][c * 128:(c + 1) * 128, pos:pos + N], o[:, 0:N], reads=[ok], writes=[("qn", c, ci)])
            for (r0, M) in ((0, 128), (128, 64)):
                bk_t = proj(384 + r0, M, N, rhs=cqn, rk="cqn", lhs=Wuq, lk="Wuq", nk=2)
                bk_s = None if isctx else proj(576 + r0, M, N, rhs=cqn, rk="cqn", lhs=Wuq, lk="Wuq", nk=2)
                o, ok = next_obf()
                rot(bk_t, bk_s, M, N, isctx, o, ok)
                s.dma("sp", g["qr_s"][r0:r0 + M, pos:pos + N], o[0:M, 0:N], reads=[ok], writes=[("qr", r0, ci)])
            for c in range(3):
                bk = pb.next()
                s.op("pe", lambda: nc.tensor.matmul(pb.bank(bk)[:, 0:N], Wukv[:, c * 128:(c + 1) * 128], ckvn[:, 0:N], start=True, stop=True),
                     reads=["Wukv", "ckvn"], writes=[bkey(bk)], psum=[bkey(bk)])
                o, ok = next_obf()
                s.op("act", lambda: nc.scalar.copy(out=o[:, 0:N], in_=pb.bank(bk)[:, 0:N]), reads=[bkey(bk)], writes=[ok], psum=[bkey(bk)])
                s.dma("sp", g["kn_s"][c * 128:(c + 1) * 128, pos:pos + N], o[:, 0:N], reads=[ok], writes=[("kn", c, ci)])
            for t in range(N // 128):
                bk = pb.next()
                bank = pb.bank(bk)
                s.op("pe", lambda: nc.tensor.matmul(bank[:, 0:384], ckvn[:, t * 128:(t + 1) * 128], Wukv[:, 384:768], start=True, stop=True),
                     reads=["ckvn", "Wukv"], writes=[bkey(bk)], psum=[bkey(bk)])
                vi = cnt["v"] % 2
                cnt["v"] += 1
                vt = vtok[vi]
                vv = vt[:, :].rearrange("p (c x) -> p c x", x=192)
                bv = bank[:, 0:384].rearrange("p (c x) -> p c x", x=128)
                s.op("act", lambda: nc.scalar.copy(out=vv[:, :, 0:64], in_=bv[:, :, 0:64]), reads=[bkey(bk)], writes=[("vtok", vi)], psum=[bkey(bk)])
                s.op("dve", lambda: nc.vector.tensor_copy(out=vv[:, :, 128:192], in_=bv[:, :, 64:128]), reads=[bkey(bk)], writes=[("vtok", vi)], psum=[bkey(bk)])
                s.dma("sp", g["vml_s"][pos + t * 128:pos + (t + 1) * 128, :], vt[:, :], reads=[("vtok", vi)], writes=[("vml", ci, t)])
        s.barrier()

    if g.get("stop_after") == "P1":
        return
    p2_lru(nc, s, pb, sb, g)
    if g.get("stop_after") == "P2":
        return
    p34_attn(nc, s, pb, sb, g, da=True)
    p34_attn(nc, s, pb, sb, g, da=False)
    if g.get("stop_after") == "P4":
        return
    p5_ffn(nc, s, pb, sb, g)


def p2_lru(nc, s, pb, sb, g):
    b, l = g["b"], g["l"]
    chunks, ntok, XBW = g["chunks"], g["ntok"], g["XBW"]
    small, small2, coef = g["small"], g["small2"], g["coef"]
    fm = g["fm"]
    hscr = [g["hf_s"], g["hb_s"]]
    with ExitStack() as ps:
        LW = sb(ps, "LW", [128, 8, 128], BF16)
        s.dma("sp", LW[:], g["lruw_b"][l], writes=["LW"], after=g["wtok"](g["lruw_b"], l))
        T = []
        for d in range(2):
            T.append(dict(
                X=[sb(ps, f"X{d}{i}", [128, 2, 515]) for i in range(2)],
                Y=[sb(ps, f"Y{d}{i}", [128, 2, 512]) for i in range(2)], Yb=[sb(ps, f"Yb{d}{i}", [128, 2, 512], BF16) for i in range(2)],
                R=[sb(ps, f"R{d}{i}", [128, 2, 512]) for i in range(2)], I=[sb(ps, f"I{d}{i}", [128, 2, 512]) for i in range(2)],
                A=[sb(ps, f"A{d}{i}", [128, 2, 512]) for i in range(2)], A2=[sb(ps, f"A2{d}{i}", [128, 2, 512]) for i in range(2)],
                Uu=[sb(ps, f"Uu{d}{i}", [128, 2, 512]) for i in range(2)],
                H=[sb(ps, f"H{d}{i}", [128, 2, 512]) for i in range(2)]))
        HF = [sb(ps, f"HF{i}", [128, 2, 512]) for i in range(2)]
        HB = [sb(ps, f"HB{i}", [128, 2, 512]) for i in range(2)]
        GG = [sb(ps, f"GG{i}", [128, 2, 512]) for i in range(2)]
        OB = [sb(ps, f"OB{i}", [128, 2, 512], BF16) for i in range(2)]
        nch = len(chunks)
        order = [list(range(nch)), [0] + list(range(nch - 1, 0, -1))]
        prev = [None, None]

        def step_all(step):
            info = []
            for dd in range(2):
                ci = order[dd][step]
                pos, N, isctx = chunks[ci]
                t = T[dd]
                xi = step % 2
                x = t["X"][xi]
                xk = ("X", dd, xi)
                off = 0 if isctx else pos + 3
                s.dma("sp", x[:, :, 0:N + 3], fm(g["xb_s"], 2)[:, :, off:off + N + 3], writes=[xk])
                info.append((dd, ci, pos, N, t, xi, x, xk))
            chains = [(i_, c) for i_ in info for c in range(2)]
            for (dd, ci, pos, N, t, xi, x, xk), c in chains:
                Y = t["Y"][xi]
                s.op("pool", lambda: nc.gpsimd.tensor_scalar(out=Y[:, c, 0:N], in0=x[:, c, 0:N], scalar1=small2[:, l, c:c + 1],
                                                              scalar2=small[:, l, 35 + c:36 + c], op0=ALU.mult, op1=ALU.add),
                     reads=[xk, "small", "small2"], writes=[("Y", dd, xi, c)])
            for k in range(1, 4):
                for (dd, ci, pos, N, t, xi, x, xk), c in chains:
                    Y = t["Y"][xi]
                    s.op("dve", lambda: nc.vector.scalar_tensor_tensor(out=Y[:, c, 0:N], in0=x[:, c, k:k + N], scalar=small2[:, l, 2 * k + c:2 * k + c + 1],
                                                                        in1=Y[:, c, 0:N], op0=ALU.mult, op1=ALU.add),
                         reads=[xk, "small2", ("Y", dd, xi, c)], writes=[("Y", dd, xi, c)])
            for (dd, ci, pos, N, t, xi, x, xk), c in chains:
                Y, Yb = t["Y"][xi], t["Yb"][xi]
                s.op("act", lambda: nc.scalar.copy(out=Yb[:, c, 0:N], in_=Y[:, c, 0:N]), reads=[("Y", dd, xi, c)], writes=[("Yb", dd, xi, c)])
            bkm = {}
            for (dd, ci, pos, N, t, xi, x, xk), c in chains:
                Yb = t["Yb"][xi]
                for ai in range(2):
                    bk = pb.next()
                    bkm[(dd, c, ai)] = bk
                    s.op("pe", lambda: nc.tensor.matmul(pb.bank(bk)[:, 0:N], LW[:, dd * 4 + c * 2 + ai, :], Yb[:, c, 0:N], start=True, stop=True),
                         reads=["LW", ("Yb", dd, xi, c)], writes=[bkey(bk)], psum=[bkey(bk)])
            for (dd, ci, pos, N, t, xi, x, xk), c in chains:
                for (nm, ai, bo) in (("R", 0, 8), ("I", 1, 12)):
                    dst = t[nm][xi]
                    bk_ = bkm[(dd, c, ai)]
                    s.op("act", lambda: nc.scalar.activation(out=dst[:, c, 0:N], in_=pb.bank(bk_)[:, 0:N], func=AF.Sigmoid,
                                                              bias=small2[:, l, bo + dd * 2 + c:bo + 1 + dd * 2 + c]),
                         reads=[bkey(bk_), "small2"], writes=[(nm, dd, xi, c)], psum=[bkey(bk_)])
            for (dd, ci, pos, N, t, xi, x, xk), c in chains:
                R, A, A2 = t["R"][xi], t["A"][xi], t["A2"][xi]
                s.op("act", lambda: nc.scalar.activation(out=A[:, c, 0:N], in_=R[:, c, 0:N], func=AF.Exp, scale=coef[:, l, dd * 2 + c:dd * 2 + c + 1]),
                     reads=[("R", dd, xi, c), "coef"], writes=[("A", dd, xi, c)])
                s.op("act", lambda: nc.scalar.activation(out=A2[:, c, 0:N], in_=R[:, c, 0:N], func=AF.Exp, scale=coef[:, l, 4 + dd * 2 + c:5 + dd * 2 + c]),
                     reads=[("R", dd, xi, c), "coef"], writes=[("A2", dd, xi, c)])
            for (dd, ci, pos, N, t, xi, x, xk), c in chains:
                A2 = t["A2"][xi]
                s.op("act", lambda: nc.scalar.activation(out=A2[:, c, 0:N], in_=A2[:, c, 0:N], func=AF.Ln, scale=-1.0, bias=1.0),
                     reads=[("A2", dd, xi, c)], writes=[("A2", dd, xi, c)])
            for (dd, ci, pos, N, t, xi, x, xk), c in chains:
                A2, I, Y = t["A2"][xi], t["I"][xi], t["Y"][xi]
                s.op("act", lambda: nc.scalar.activation(out=A2[:, c, 0:N], in_=A2[:, c, 0:N], func=AF.Exp, scale=0.5),
                     reads=[("A2", dd, xi, c)], writes=[("A2", dd, xi, c)])
                s.op("pool", lambda: nc.gpsimd.tensor_tensor(out=I[:, c, 0:N], in0=I[:, c, 0:N], in1=Y[:, c, 0:N], op=ALU.mult),
                     reads=[("I", dd, xi, c), ("Y", dd, xi, c)], writes=[("I", dd, xi, c)])
            for (dd, ci, pos, N, t, xi, x, xk), c in chains:
                A2, I, Uu = t["A2"][xi], t["I"][xi], t["Uu"][xi]
                s.op("pool", lambda: nc.gpsimd.tensor_tensor(out=Uu[:, c, 0:N], in0=I[:, c, 0:N], in1=A2[:, c, 0:N], op=ALU.mult),
                     reads=[("I", dd, xi, c), ("A2", dd, xi, c)], writes=[("Uu", dd, xi, c)])
            hi = step % 2
            for (dd, ci, pos, N, t, xi, x, xk), c in chains:
                A, Uu = t["A"][xi], t["Uu"][xi]
                h = t["H"][hi]
                hk = ("H", dd, hi, c)
                if prev[dd] is None:
                    init = 0.0
                    rk = []
                else:
                    phi, pN = prev[dd]
                    init = t["H"][phi][:, c, pN - 1:pN] if dd == 0 else t["H"][phi][:, c, 0:1]
                    rk = [("H", dd, phi, c)]
                if dd == 0:
                    s.op("dve", lambda: nc.vector.tensor_tensor_scan(out=h[:, c, 0:N], data0=A[:, c, 0:N], data1=Uu[:, c, 0:N], initial=init,
                                                                      op0=ALU.mult, op1=ALU.add),
                         reads=[("A", dd, xi, c), ("Uu", dd, xi, c)] + rk, writes=[hk])
                else:
                    s.op("dve", lambda: nc.vector.tensor_tensor_scan(out=h[:, c, 0:N][:, ::-1], data0=A[:, c, 0:N][:, ::-1], data1=Uu[:, c, 0:N][:, ::-1],
                                                                      initial=init, op0=ALU.mult, op1=ALU.add),
                         reads=[("A", dd, xi, c), ("Uu", dd, xi, c)] + rk, writes=[hk])
            for (dd, ci, pos, N, t, xi, x, xk) in info:
                s.dma("sp", fm(hscr[dd], 2)[:, :, pos:pos + N], t["H"][hi][:, :, 0:N], reads=[("H", dd, hi, 0), ("H", dd, hi, 1)], writes=[("hs", dd, ci)])
                prev[dd] = (hi, N)

        for step in range(nch):
            step_all(step)
        for ci, (pos, N, isctx) in enumerate(chunks):
            j = ci % 2
            s.dma("sp", HF[j][:, :, 0:N], fm(g["hf_s"], 2)[:, :, pos:pos + N], reads=[("hs", 0, ci)], writes=[("HF", j)])
            s.dma("sp", HB[j][:, :, 0:N], fm(g["hb_s"], 2)[:, :, pos:pos + N], reads=[("hs", 1, ci)], writes=[("HB", j)])
            s.dma("sp", GG[j][:, :, 0:N], fm(g["gg_s"], 2)[:, :, pos:pos + N], writes=[("GG", j)])
            e1 = "dve" if j == 0 else "pool"
            eng1 = nc.vector if j == 0 else nc.gpsimd
            s.op(e1, lambda: eng1.tensor_tensor(out=HF[j][:, :, 0:N], in0=HF[j][:, :, 0:N], in1=HB[j][:, :, 0:N], op=ALU.add),
                 reads=[("HF", j), ("HB", j)], writes=[("HF", j)])
            s.op(e1, lambda: eng1.tensor_tensor(out=OB[j][:, :, 0:N], in0=HF[j][:, :, 0:N], in1=GG[j][:, :, 0:N], op=ALU.mult),
                 reads=[("HF", j), ("GG", j)], writes=[("OB", j)])
            s.dma("sp", fm(g["aT_s"], 2)[:, :, pos:pos + N], OB[j][:, :, 0:N], reads=[("OB", j)], writes=[("aT", ci)])
        s.barrier()


def p34_attn(nc, s, pb, sb, g, da):
    b, l, need_ctx = g["b"], g["l"], g["need_ctx"]
    chunks, ntok, nkb = g["chunks"], g["ntok"], g["nkb"]
    negl, gn, small, ones_b = g["negl"], g["gn"], g["small"], g["ones_b"]
    fm = g["fm"]
    with ExitStack() as ps:
        if da:
            K = sb(ps, "K", [128, 3, ntok], BF16)
            Q = [sb(ps, f"Q{i}", [128, 3, 4, 512], BF16) for i in range(2)]
        else:
            K = sb(ps, "K", [96, 6, ntok], BF16)
            Q = [sb(ps, f"Q{i}", [96, 6, 512], BF16) for i in range(2)]
        VA = sb(ps, "VA", [128, nkb, 576], BF16)
        PT = [sb(ps, f"PT{i}", [128, 2, 512], BF16) for i in range(2)]
        R1 = sb(ps, "R1", [128, 512])
        R2 = sb(ps, "R2", [128, 512])
        Aa = sb(ps, "Aa", [128, 512])
        Bb = sb(ps, "Bb", [128, 512])
        Dd = sb(ps, "Dd", [128, 512])
        Dq = sb(ps, "Dq", [128, 512], BF16)
        Rs = sb(ps, "Rs", [128, 512])
        OT = [sb(ps, f"OT{i}", [128, 512], BF16) for i in range(2)]
        if da:
            for i in range(2):
                s.op("pool", lambda: nc.gpsimd.memset(Q[i][:], 0.0), writes=[("Q", i)])
            if g.get("conv_next") is not None:
                g["conv_next"]()
            s.dma("sp", K[:], fm(g["kda_s"], 3), writes=["K"])
            s.dma("sp", VA[:], g["vda_s"].rearrange("(k p) d -> p k d", p=128), writes=["VA"])
        else:
            s.dma("sp", K[0:64, :, :], g["kn_s"].rearrange("(h p) t -> p h t", p=64), writes=["K"])
            for h in range(6):
                s.dma("sp", K[64:96, h, :], g["kr_s"][:, :], writes=["K"])
            s.dma("sp", VA[:], g["vml_s"].rearrange("(k p) d -> p k d", p=128), writes=["VA"])
        act_chunks = [(ci, pos, N, isctx) for ci, (pos, N, isctx) in enumerate(chunks) if not (isctx and not need_ctx)]
        nmaps = 2 if da else 1
        units = []
        o_rr = 0
        ot_rr = 0
        for k_, (ci, pos, N, isctx) in enumerate(act_chunks):
            kbs = [0, 1] if isctx else list(range(nkb))
            for c in range(3):
                oti = ot_rr % 2
                ot_rr += 1
                for hh in range(2):
                    obk = []
                    for m in range(nmaps):
                        ob = 4 + (o_rr % 3)
                        o_rr += 1
                        obk.append(ob)
                        npair = len(kbs) // 2
                        for pi in range(npair):
                            units.append(dict(k=k_, ci=ci, pos=pos, N=N, c=c, hh=hh, m=m, ob=ob, obk=list(obk), oti=oti,
                                              kb=(kbs[2 * pi], kbs[2 * pi + 1]), first=(pi == 0), last=(pi == npair - 1),
                                              head_done=(pi == npair - 1 and m == nmaps - 1), pair_done=(pi == npair - 1 and m == nmaps - 1 and hh == 1)))

        def load_q(k_):
            ci, pos, N, isctx = act_chunks[k_]
            q = Q[k_ % 2]
            qk = ("Q", k_ % 2)
            if da:
                for j in range(4):
                    s.dma("sp", q[j * 32:(j + 1) * 32, :, j, 0:N], fm(g["qda_s"], 3)[j * 32:(j + 1) * 32, :, pos:pos + N], writes=[qk])
            else:
                s.dma("sp", q[0:64, :, 0:N], g["qn_s"].rearrange("(h p) t -> p h t", p=64)[:, :, pos:pos + N], writes=[qk])
                s.dma("sp", q[64:96, :, 0:N], g["qr_s"].rearrange("(h p) t -> p h t", p=32)[:, :, pos:pos + N], writes=[qk])

        def emit_qk(i):
            u = units[i]
            N, c, hh, m = u["N"], u["c"], u["hh"], u["m"]
            q = Q[u["k"] % 2]
            qk = ("Q", u["k"] % 2)
            sp_i = i % 2
            Sp = pb.pair(sp_i)
            sk = [bkey(2 * sp_i), bkey(2 * sp_i + 1)]
            h = 2 * c + hh
            for j in range(2):
                kb = u["kb"][j]
                if da:
                    s.op("pe", lambda: nc.tensor.matmul(Sp[:, j, 0:N], K[:, c, kb * 128:(kb + 1) * 128],
                                                        q[:, c, hh * 2 + m, 0:N], start=True, stop=True),
                         reads=["K", qk], writes=[sk[j]], psum=[sk[j]], signal=(j == 1))
                else:
                    s.op("pe", lambda: nc.tensor.matmul(Sp[:, j, 0:N], K[0:96, h, kb * 128:(kb + 1) * 128],
                                                        q[0:96, h, 0:N], start=True, stop=True),
                         reads=["K", qk], writes=[sk[j]], psum=[sk[j]], signal=(j == 1))

        def emit_exp(i):
            u = units[i]
            N = u["N"]
            sp_i = i % 2
            Sp = pb.pair(sp_i)
            sk = [bkey(2 * sp_i), bkey(2 * sp_i + 1)]
            s.op("act", lambda: nc.scalar.activation(out=PT[sp_i][:, :, 0:N], in_=Sp[:, :, 0:N], func=AF.Exp,
                                                      scale=(DA_SCALE if da else MLA_SCALE)),
                 reads=sk, writes=[("PT", sp_i)], psum=sk)

        def emit_pv(i):
            u = units[i]
            N, c, hh = u["N"], u["c"], u["hh"]
            sp_i = i % 2
            ob = u["ob"]
            vcol = c * 192 + hh * 64
            for j in range(2):
                kb = u["kb"][j]
                s.op("pe", lambda: nc.tensor.matmul(pb.bank(ob)[:, 0:N], VA[:, kb, vcol:vcol + 128], PT[sp_i][:, j, 0:N],
                                                    start=(u["first"] and j == 0), stop=(u["last"] and j == 1)),
                     reads=["VA", ("PT", sp_i)], writes=[bkey(ob)], psum=[bkey(ob)], signal=(j == 1))

        def epi_a(u):
            N, hh = u["N"], u["hh"]
            nb_ = hh * 64
            nsl = slice(nb_, nb_ + 64)
            dsl = slice(64 - nb_, 128 - nb_)
            obk = u["obk"]
            ot = OT[u["oti"]]
            otk = ("OT", u["oti"])
            O1 = pb.bank(obk[0])
            s.op("dve", lambda: nc.vector.reciprocal(out=R1[nsl, 0:N], in_=O1[dsl, 0:N]), reads=[bkey(obk[0])], writes=["R1"], psum=[bkey(obk[0])])
            if not da:
                s.op("dve", lambda: nc.vector.tensor_tensor(out=ot[nsl, 0:N], in0=O1[nsl, 0:N], in1=R1[nsl, 0:N], op=ALU.mult),
                     reads=[bkey(obk[0]), "R1"], writes=[otk], psum=[bkey(obk[0])])
                return
            O2 = pb.bank(obk[1])
            s.op("dve", lambda: nc.vector.reciprocal(out=R2[nsl, 0:N], in_=O2[dsl, 0:N]), reads=[bkey(obk[1])], writes=["R2"], psum=[bkey(obk[1])])
            s.op("dve", lambda: nc.vector.tensor_tensor(out=Aa[nsl, 0:N], in0=O1[nsl, 0:N], in1=R1[nsl, 0:N], op=ALU.mult),
                 reads=[bkey(obk[0]), "R1"], writes=["Aa"], psum=[bkey(obk[0])])
            s.op("dve", lambda: nc.vector.tensor_tensor(out=Bb[nsl, 0:N], in0=O2[nsl, 0:N], in1=R2[nsl, 0:N], op=ALU.mult),
                 reads=[bkey(obk[1]), "R2"], writes=["Bb"], psum=[bkey(obk[1])])
            s.op("dve", lambda: nc.vector.scalar_tensor_tensor(out=Dd[nsl, 0:N], in0=Bb[nsl, 0:N], scalar=negl[nsl, l:l + 1], in1=Aa[nsl, 0:N],
                                                                op0=ALU.mult, op1=ALU.add),
                 reads=["Aa", "Bb", "negl"], writes=[("Dd", hh)])
            s.op("dve", lambda: nc.vector.tensor_tensor(out=Dq[nsl, 0:N], in0=Dd[nsl, 0:N], in1=Dd[nsl, 0:N], op=ALU.mult),
                 reads=[("Dd", hh)], writes=[("Dq", hh)])

        def epi_b(u):
            N, hh = u["N"], u["hh"]
            nb_ = hh * 64
            nsl = slice(nb_, nb_ + 64)
            ot = OT[u["oti"]]
            otk = ("OT", u["oti"])
            rb = pb.bank(7)
            s.op("pe", lambda: nc.tensor.matmul(rb[nsl, 0:N], ones_b[nsl, 0:64], Dq[nsl, 0:N], start=True, stop=True, tile_position=(nb_, nb_)),
                 reads=["ones_b", ("Dq", hh)], writes=[bkey(7)], psum=[bkey(7)])
            s.op("act", lambda: nc.scalar.activation(out=Rs[nsl, 0:N], in_=rb[nsl, 0:N], func=AF.Ln, scale=1.0 / 64.0, bias=small[nsl, l, 38:39]),
                 reads=[bkey(7), "small"], writes=[("Rs", hh)], psum=[bkey(7)])
            s.op("act", lambda: nc.scalar.activation(out=Rs[nsl, 0:N], in_=Rs[nsl, 0:N], func=AF.Exp, scale=-0.5), reads=[("Rs", hh)], writes=[("Rs", hh)])
            s.op("dve", lambda: nc.vector.scalar_tensor_tensor(out=ot[nsl, 0:N], in0=Dd[nsl, 0:N], scalar=gn[nsl, l:l + 1], in1=Rs[nsl, 0:N],
                                                                op0=ALU.mult, op1=ALU.mult),
                 reads=[("Dd", hh), "gn", ("Rs", hh)], writes=[otk])

        def store(u):
            scr = g["bT_s"] if da else g["mT_s"]
            c, pos, N = u["c"], u["pos"], u["N"]
            s.dma("sp", scr[c * 128:(c + 1) * 128, pos:pos + N], OT[u["oti"]][:, 0:N], reads=[("OT", u["oti"])], writes=[("bm", da, c, u["ci"])])

        deferred = []
        nU = len(units)
        load_q(0)
        emit_qk(0)
        DEFER = 12
        for i in range(nU):
            u = units[i]
            if u["first"] and u["c"] == 0 and u["hh"] == 0 and u["m"] == 0 and u["k"] + 1 < len(act_chunks):
                load_q(u["k"] + 1)
            if i + 1 < nU:
                emit_qk(i + 1)
            emit_exp(i)
            emit_pv(i)
            if u["head_done"]:
                while deferred:
                    _, kind, uu = deferred.pop(0)
                    (epi_b if kind == "b" else store)(uu)
                epi_a(u)
                if da:
                    deferred.append((i + DEFER, "b", u))
                    if u["pair_done"]:
                        deferred.append((i + DEFER, "s", u))
                elif u["pair_done"]:
                    store(u)
            while deferred and deferred[0][0] <= i:
                _, kind, uu = deferred.pop(0)
                (epi_b if kind == "b" else store)(uu)
        for _, kind, uu in deferred:
            (epi_b if kind == "b" else store)(uu)
        s.barrier()


def p5_ffn(nc, s, pb, sb, g):
    b, l, need_ctx = g["b"], g["l"], g["need_ctx"]
    n_layers, chunks, ntok = g["n_layers"], g["chunks"], g["ntok"]
    mod, small, ones_f, ident, router, rbias = g["mod"], g["small"], g["ones_f"], g["ident"], g["router"], g["rbias"]
    fm, modcol = g["fm"], g["modcol"]
    last = (l == n_layers - 1)
    F32R = mybir.dt.float32r
    with ExitStack() as ps:
        Wo = sb(ps, "Wo", [128, KC, D], BF16)
        MIX = sb(ps, "MIX", [128, KC, 512], BF16)
        XR = [sb(ps, f"Xr{i}", [128, KC, 512]) for i in range(2)]
        SQ = [sb(ps, f"SQ{i}", [128, 512], F32R) for i in range(2)]
        YR = [sb(ps, f"YR{i}", [128, 512], F32R) for i in range(2)]
        ones_r = sb(ps, "ones_r", [128, 128], F32R)
        s.op("act", lambda: nc.scalar.copy(out=ones_r[:], in_=ones_f[:]), reads=["ones_f"], writes=["ones_r"])
        ST = [sb(ps, f"ST{i}", [128, 512]) for i in range(4)]
        V32 = sb(ps, "V32", [128, KC, 512])
        Vb = sb(ps, "Vb", [128, KC, 512], BF16)
        GA = sb(ps, "GA", [128, NE, 2, 512], BF16)
        W13 = [sb(ps, f"W13_{i}", [128, 2, KC, DE], BF16) for i in range(3)]
        W2 = [sb(ps, f"W2_{i}", [128, NE, 2, 128], BF16) for i in range(3)]
        SC = sb(ps, "SC", [128, 4, NE])
        SEL = sb(ps, "SEL", [128, 4, NE])
        SEL2 = sb(ps, "SEL2", [128, 4, NE])
        EQ = sb(ps, "EQ", [128, 4, NE])
        M1 = sb(ps, "M1", [128, 16])
        M2 = sb(ps, "M2", [128, 16])
        GS = sb(ps, "GS", [128, 16])
        GM = sb(ps, "GM", [128, 4])
        ING = sb(ps, "ING", [128, 16])
        GT = sb(ps, "GT", [128, 4, NE])
        DEN = sb(ps, "DEN", [128, 4])
        GTT = sb(ps, "GTT", [16, 512], BF16)
        SELM = sb(ps, "SELM", [16, NE, 128], BF16)
        SIL = [sb(ps, f"SIL{i}", [128, 512]) for i in range(2)]
        HH = [sb(ps, f"HH{i}", [128, 512]) for i in range(2)]
        OTK = V32[:, :, :].rearrange("p (t h) n -> p t (h n)", h=2)
        V32K = [("V32", kc) for kc in range(KC)]
        s.dma("sp", Wo[:], g["wout_b"][l], writes=["Wo"], after=g["wtok"](g["wout_b"], l))
        s.dma("sp", SELM[:], g["selm_in"][:, :, :], writes=["SELM"])
        wrr = {"w13": 0, "w2": 0, "hb": 0}
        act = [(ci, pos, N, isctx) for ci, (pos, N, isctx) in enumerate(chunks) if not (isctx and not need_ctx)]

        def XK(i):
            return [("Xr", i % 2, kc) for kc in range(KC)]

        def A_mix(i):
            ci, pos, N, isctx = act[i]
            s.dma("sp", MIX[:, 0:2, 0:N], fm(g["aT_s"], 2)[:, :, pos:pos + N], writes=["MIX"])
            s.dma("sp", MIX[:, 2:5, 0:N], fm(g["bT_s"], 3)[:, :, pos:pos + N], writes=["MIX"])
            s.dma("sp", MIX[:, 5:8, 0:N], fm(g["mT_s"], 3)[:, :, pos:pos + N], writes=["MIX"])

        def A_x(i):
            ci, pos, N, isctx = act[i]
            s.dma("sp", XR[i % 2][:, :, 0:N], fm(g["xT_s"], KC)[:, :, pos:pos + N], reads=[("xTw", ci)], writes=XK(i))

        def B(i):
            ci, pos, N, isctx = act[i]
            bi = 2 if isctx else b
            Xr = XR[i % 2]
            for oc in range(KC):
                bk = pb.next()
                for kc in range(KC):
                    s.op("pe", lambda: nc.tensor.matmul(pb.bank(bk)[:, 0:N], Wo[:, kc, oc * 128:(oc + 1) * 128], MIX[:, kc, 0:N],
                                                        start=(kc == 0), stop=(kc == KC - 1)),
                         reads=["Wo", "MIX"], writes=[bkey(bk)], psum=[bkey(bk)], signal=(kc == KC - 1))
                s.op("dve", lambda: nc.vector.scalar_tensor_tensor(out=Xr[:, oc, 0:N], in0=pb.bank(bk)[:, 0:N], scalar=modcol(l, 2, oc, bi),
                                                                    in1=Xr[:, oc, 0:N], op0=ALU.mult, op1=ALU.add),
                     reads=[bkey(bk), "mod", ("Xr", i % 2, oc)], writes=[("Xr", i % 2, oc)], psum=[bkey(bk)])

        def ln_stats(i):
            ci, pos, N, isctx = act[i]
            Xr = XR[i % 2]
            bk_m = pb.next()
            for kc in range(KC):
                yr = YR[kc % 2]
                yrk = ("YR", kc % 2)
                if kc % 2 == 0:
                    s.op("act", lambda: nc.scalar.copy(out=yr[:, 0:N], in_=Xr[:, kc, 0:N]), reads=[("Xr", i % 2, kc)], writes=[yrk])
                else:
                    s.op("dve", lambda: nc.vector.tensor_copy(out=yr[:, 0:N], in_=Xr[:, kc, 0:N]), reads=[("Xr", i % 2, kc)], writes=[yrk])
                s.op("pe", lambda: nc.tensor.matmul(pb.bank(bk_m)[:, 0:N], ones_r[:, :], yr[:, 0:N],
                                                    start=(kc == 0), stop=(kc == KC - 1)),
                     reads=["ones_r", yrk], writes=[bkey(bk_m)], psum=[bkey(bk_m)], signal=True)
            bk_q = pb.next()
            for kc in range(KC):
                sq_ = SQ[kc % 2]
                sqk = ("SQ", kc % 2)
                if kc % 2 == 0:
                    s.op("act", lambda: nc.scalar.activation(out=sq_[:, 0:N], in_=Xr[:, kc, 0:N], func=AF.Square), reads=[("Xr", i % 2, kc)], writes=[sqk])
                else:
                    s.op("pool", lambda: nc.gpsimd.tensor_tensor(out=sq_[:, 0:N], in0=Xr[:, kc, 0:N], in1=Xr[:, kc, 0:N], op=ALU.mult),
                         reads=[("Xr", i % 2, kc)], writes=[sqk])
                s.op("pe", lambda: nc.tensor.matmul(pb.bank(bk_q)[:, 0:N], ones_r[:, :], sq_[:, 0:N],
                                                    start=(kc == 0), stop=(kc == KC - 1)),
                     reads=["ones_r", sqk], writes=[bkey(bk_q)], psum=[bkey(bk_q)], signal=True)
            mean = pb.bank(bk_m)
            ey2 = pb.bank(bk_q)
            m2, var, rs, nmr = ST[0], ST[1], ST[2], ST[3]
            s.op("act", lambda: nc.scalar.activation(out=m2[:, 0:N], in_=mean[:, 0:N], func=AF.Square), reads=[bkey(bk_m)], writes=["ST0"], psum=[bkey(bk_m)])
            s.op("dve", lambda: nc.vector.tensor_tensor(out=var[:, 0:N], in0=ey2[:, 0:N], in1=m2[:, 0:N], op=ALU.subtract),
                 reads=[bkey(bk_q), "ST0"], writes=["ST1"], psum=[bkey(bk_q)])
            s.op("act", lambda: nc.scalar.activation(out=var[:, 0:N], in_=var[:, 0:N], func=AF.Ln, bias=small[:, l, 39:40]), reads=["ST1", "small"], writes=["ST1"])
            s.op("act", lambda: nc.scalar.activation(out=rs[:, 0:N], in_=var[:, 0:N], func=AF.Exp, scale=-0.5), reads=["ST1"], writes=["ST2"])
            s.op("dve", lambda: nc.vector.scalar_tensor_tensor(out=nmr[:, 0:N], in0=mean[:, 0:N], scalar=-1.0, in1=rs[:, 0:N], op0=ALU.mult, op1=ALU.mult),
                 reads=[bkey(bk_m), "ST2"], writes=["ST3"], psum=[bkey(bk_m)])

        def ln_norm(i, gcol, bcol):
            ci, pos, N, isctx = act[i]
            Xr = XR[i % 2]
            rs, nmr = ST[2], ST[3]
            for kc in range(KC):
                e1 = "dve" if kc % 2 == 0 else "pool"
                eng1 = nc.vector if e1 == "dve" else nc.gpsimd
                xk = ("Xr", i % 2, kc)
                s.op(e1, lambda: eng1.tensor_tensor(out=Xr[:, kc, 0:N], in0=Xr[:, kc, 0:N], in1=rs[:, 0:N], op=ALU.mult), reads=[xk, "ST2"], writes=[xk])
                s.op(e1, lambda: eng1.tensor_tensor(out=Xr[:, kc, 0:N], in0=Xr[:, kc, 0:N], in1=nmr[:, 0:N], op=ALU.add), reads=[xk, "ST3"], writes=[xk])
                s.op("act", lambda: nc.scalar.activation(out=Xr[:, kc, 0:N], in_=Xr[:, kc, 0:N], func=AF.Identity,
                                                          scale=small[:, l, gcol + kc:gcol + kc + 1], bias=small[:, l, bcol + kc:bcol + kc + 1]),
                     reads=[xk, "small"], writes=[xk])

        def C2(i):
            ci, pos, N, isctx = act[i]
            bi = 2 if isctx else b
            Xr = XR[i % 2]
            ln_norm(i, 0, 8)
            for kc in range(KC):
                s.op("dve", lambda: nc.vector.tensor_scalar(out=V32[:, kc, 0:N], in0=Xr[:, kc, 0:N], scalar1=modcol(l, 4, kc, bi),
                                                             scalar2=modcol(l, 3, kc, bi), op0=ALU.mult, op1=ALU.add),
                     reads=[("Xr", i % 2, kc), "mod"], writes=[("V32", kc)])
                if kc % 2 == 0:
                    s.op("act", lambda: nc.scalar.copy(out=Vb[:, kc, 0:N], in_=V32[:, kc, 0:N]), reads=[("V32", kc)], writes=["Vb"])
                else:
                    s.op("pool", lambda: nc.gpsimd.tensor_copy(out=Vb[:, kc, 0:N], in_=V32[:, kc, 0:N]), reads=[("V32", kc)], writes=["Vb"])

        def C3a(i):
            ci, pos, N, isctx = act[i]
            nt = N // 128
            bk_r = pb.next()
            rbk = pb.bank(bk_r)
            for t in range(nt):
                for kc in range(KC):
                    s.op("pe", lambda: nc.tensor.matmul(rbk[:, t * NE:(t + 1) * NE], V32[:, kc, t * 128:(t + 1) * 128], router[:, kc, :],
                                                        start=(kc == 0), stop=(kc == KC - 1)),
                         reads=V32K + ["router"], writes=[bkey(bk_r)], psum=[bkey(bk_r)], signal=(kc == KC - 1))
            T4 = [128, nt, NE]
            s.op("act", lambda: nc.scalar.activation(out=SC[:, 0:nt, :], in_=rbk[:, 0:nt * NE].rearrange("p (t e) -> p t e", e=NE), func=AF.Sigmoid),
                 reads=[bkey(bk_r)], writes=["SC"], psum=[bkey(bk_r)])
            s.op("dve", lambda: nc.vector.tensor_tensor(out=SEL[:, 0:nt, :], in0=SC[:, 0:nt, :], in1=rbias[:, :].unsqueeze(1).to_broadcast(T4), op=ALU.add),
                 reads=["SC", "rbias"], writes=["SEL"])
            ng = nt * 4
            sel4 = SEL[:, 0:nt, :].rearrange("p t (g k) -> p (t g) k", k=4)
            sel24 = SEL2[:, 0:nt, :].rearrange("p t (g k) -> p (t g) k", k=4)
            eq4 = EQ[:, 0:nt, :].rearrange("p t (g k) -> p (t g) k", k=4)
            s.op("dve", lambda: nc.vector.tensor_reduce(out=M1[:, 0:ng], in_=sel4, axis=AX.X, op=ALU.max), reads=["SEL"], writes=["M1"])
            s.op("dve", lambda: nc.vector.tensor_tensor(out=eq4, in0=sel4, in1=M1[:, 0:ng].unsqueeze(2).to_broadcast([128, ng, 4]), op=ALU.is_equal),
                 reads=["SEL", "M1"], writes=["EQ"])
            s.op("dve", lambda: nc.vector.scalar_tensor_tensor(out=sel24, in0=eq4, scalar=-1e9, in1=sel4, op0=ALU.mult, op1=ALU.add),
                 reads=["EQ", "SEL"], writes=["SEL2"])
            s.op("dve", lambda: nc.vector.tensor_reduce(out=M2[:, 0:ng], in_=sel24, axis=AX.X, op=ALU.max), reads=["SEL2"], writes=["M2"])
            s.op("dve", lambda: nc.vector.tensor_tensor(out=GS[:, 0:ng], in0=M1[:, 0:ng], in1=M2[:, 0:ng], op=ALU.add), reads=["M1", "M2"], writes=["GS"])
            gs3 = GS[:, 0:ng].rearrange("p (t g) -> p t g", g=4)
            s.op("dve", lambda: nc.vector.tensor_reduce(out=GM[:, 0:nt], in_=gs3, axis=AX.X, op=ALU.max), reads=["GS"], writes=["GM"])
            s.op("dve", lambda: nc.vector.tensor_tensor(out=ING[:, 0:ng].rearrange("p (t g) -> p t g", g=4), in0=gs3,
                                                        in1=GM[:, 0:nt].unsqueeze(2).to_broadcast([128, nt, 4]), op=ALU.is_equal),
                 reads=["GS", "GM"], writes=["ING"])
            s.op("dve", lambda: nc.vector.tensor_tensor(out=eq4, in0=sel4, in1=M2[:, 0:ng].unsqueeze(2).to_broadcast([128, ng, 4]), op=ALU.is_ge),
                 reads=["SEL", "M2"], writes=["EQ"])
            s.op("dve", lambda: nc.vector.tensor_tensor(out=eq4, in0=eq4, in1=ING[:, 0:ng].unsqueeze(2).to_broadcast([128, ng, 4]), op=ALU.mult),
                 reads=["EQ", "ING"], writes=["EQ"])
            s.op("dve", lambda: nc.vector.tensor_tensor(out=GT[:, 0:nt, :], in0=EQ[:, 0:nt, :], in1=SC[:, 0:nt, :], op=ALU.mult),
                 reads=["EQ", "SC"], writes=["GT"])
            s.op("dve", lambda: nc.vector.tensor_reduce(out=DEN[:, 0:nt], in_=GT[:, 0:nt, :], axis=AX.X, op=ALU.add), reads=["GT"], writes=["DEN"])
            s.op("dve", lambda: nc.vector.reciprocal(out=DEN[:, 0:nt], in_=DEN[:, 0:nt]), reads=["DEN"], writes=["DEN"])
            s.op("dve", lambda: nc.vector.tensor_tensor(out=GT[:, 0:nt, :], in0=GT[:, 0:nt, :], in1=DEN[:, 0:nt].unsqueeze(2).to_broadcast(T4), op=ALU.mult),
                 reads=["GT", "DEN"], writes=["GT"])

        def C3b(i):
            ci, pos, N, isctx = act[i]
            nt = N // 128
            bk_t = pb.next()
            for t in range(nt):
                s.op("pe", lambda: nc.tensor.transpose(pb.bank(bk_t)[0:NE, t * 128:(t + 1) * 128], GT[:, t, :], ident[:]),
                     reads=["GT", "ident"], writes=[bkey(bk_t)], psum=[bkey(bk_t)], signal=(t == nt - 1))
            s.op("act", lambda: nc.scalar.copy(out=GTT[:, 0:N], in_=pb.bank(bk_t)[0:NE, 0:N]), reads=[bkey(bk_t)], writes=["GTT"], psum=[bkey(bk_t)])

        def E(i):
            ci, pos, N, isctx = act[i]
            for e in range(NE):
                wi = wrr["w13"] % 3
                wrr["w13"] += 1
                w13 = W13[wi]
                wk = ("W13", wi)
                s.dma("sp", w13[:], g["w13_b"][l, e], writes=[wk], after=[g["wtok"](g["w13_b"], l)[e]])
                bk_g = pb.next()
                s.op("pe", lambda: nc.tensor.matmul(pb.bank(bk_g)[:, 0:N], SELM[:, e, :], GTT[:, 0:N], start=True, stop=True),
                     reads=["SELM", "GTT"], writes=[bkey(bk_g)], psum=[bkey(bk_g)])
                for hc in range(2):
                    bk1 = pb.next()
                    for kc in range(KC):
                        s.op("pe", lambda: nc.tensor.matmul(pb.bank(bk1)[:, 0:N], w13[:, 0, kc, hc * 128:(hc + 1) * 128], Vb[:, kc, 0:N],
                                                            start=(kc == 0), stop=(kc == KC - 1)),
                             reads=[wk, "Vb"], writes=[bkey(bk1)], psum=[bkey(bk1)], signal=(kc == KC - 1))
                    bk3 = pb.next()
                    for kc in range(KC):
                        s.op("pe", lambda: nc.tensor.matmul(pb.bank(bk3)[:, 0:N], w13[:, 1, kc, hc * 128:(hc + 1) * 128], Vb[:, kc, 0:N],
                                                            start=(kc == 0), stop=(kc == KC - 1)),
                             reads=[wk, "Vb"], writes=[bkey(bk3)], psum=[bkey(bk3)], signal=(kc == KC - 1))
                    hi = wrr["hb"] % 2
                    wrr["hb"] += 1
                    s.op("act", lambda: nc.scalar.activation(out=SIL[hi][:, 0:N], in_=pb.bank(bk1)[:, 0:N], func=AF.Silu),
                         reads=[bkey(bk1)], writes=[("SIL", hi)], psum=[bkey(bk1)])
                    s.op("dve", lambda: nc.vector.tensor_tensor(out=HH[hi][:, 0:N], in0=pb.bank(bk3)[:, 0:N], in1=SIL[hi][:, 0:N], op=ALU.mult),
                         reads=[bkey(bk3), ("SIL", hi)], writes=[("HH", hi)], psum=[bkey(bk3)])
                    s.op("dve", lambda: nc.vector.tensor_tensor(out=GA[:, e, hc, 0:N], in0=pb.bank(bk_g)[:, 0:N], in1=HH[hi][:, 0:N], op=ALU.mult),
                         reads=[bkey(bk_g), ("HH", hi)], writes=[("GA", e, hc)], psum=[bkey(bk_g)])

        def Dh(i, half):
            ci, pos, N, isctx = act[i]
            bi = 2 if isctx else b
            Xr = XR[i % 2]
            for oc in range(half * 4, half * 4 + 4):
                wi = wrr["w2"] % 3
                wrr["w2"] += 1
                w2 = W2[wi]
                wk = ("W2", wi)
                s.dma("sp", w2[:], g["w2_b"][l, oc], writes=[wk], after=[g["wtok"](g["w2_b"], l)[oc]])
                bk = pb.next()
                for e in range(NE):
                    for hc in range(2):
                        s.op("pe", lambda: nc.tensor.matmul(pb.bank(bk)[:, 0:N], w2[:, e, hc, :], GA[:, e, hc, 0:N],
                                                            start=(e == 0 and hc == 0), stop=(e == NE - 1 and hc == 1)),
                             reads=[wk, ("GA", e, hc)], writes=[bkey(bk)], psum=[bkey(bk)], signal=(e == NE - 1 and hc == 1))
                s.op("dve", lambda: nc.vector.scalar_tensor_tensor(out=Xr[:, oc, 0:N], in0=pb.bank(bk)[:, 0:N], scalar=modcol(l, 5, oc, bi),
                                                                    in1=Xr[:, oc, 0:N], op0=ALU.mult, op1=ALU.add),
                     reads=[bkey(bk), "mod", ("Xr", i % 2, oc)], writes=[("Xr", i % 2, oc)], psum=[bkey(bk)])

        def D3(i):
            ci, pos, N, isctx = act[i]
            nt = N // 128
            Xr = XR[i % 2]
            ln_stats(i)
            ln_norm(i, 16, 24)
            if not last:
                s.dma("sp", fm(g["xT_s"], KC)[:, :, pos:pos + N], Xr[:, :, 0:N], reads=XK(i), writes=[("xTw", ci)])
            else:
                for t in range(nt):
                    for half in range(2):
                        bk = pb.next()
                        for j in range(4):
                            kc = half * 4 + j
                            s.op("pe", lambda: nc.tensor.transpose(pb.bank(bk)[:, j * 128:(j + 1) * 128], Xr[:, kc, t * 128:(t + 1) * 128], ident[:]),
                                 reads=[("Xr", i % 2, kc), "ident"], writes=[bkey(bk)], psum=[bkey(bk)], signal=(j == 3))
                        if half == 0:
                            s.op("act", lambda: nc.scalar.copy(out=OTK[:, t, 0:512], in_=pb.bank(bk)[:, :]), reads=[bkey(bk)], writes=V32K, psum=[bkey(bk)])
                        else:
                            s.op("dve", lambda: nc.vector.tensor_copy(out=OTK[:, t, 512:1024], in_=pb.bank(bk)[:, :]), reads=[bkey(bk)], writes=V32K, psum=[bkey(bk)])
                lp = pos - CTX
                s.dma("sp", g["out_d"][b, lp:lp + N, :].rearrange("(t p) d -> p t d", p=128), OTK[:, 0:nt, :], reads=V32K, writes=[("out", ci)])

        n = len(act)
        A_mix(0)
        A_x(0)
        for i in range(n):
            B(i)
            if i + 1 < n:
                A_mix(i + 1)
            ln_stats(i)
            C2(i)
            if i > 0:
                Dh(i - 1, 0)
            C3a(i)
            if i > 0:
                Dh(i - 1, 1)
            C3b(i)
            if i > 0:
                D3(i - 1)
            if i + 1 < n:
                A_x(i + 1)
            E(i)
        Dh(n - 1, 0)
        Dh(n - 1, 1)
        D3(n - 1)
        s.barrier()


def _swap32(a):
    sh = a.shape
    a = a.reshape(sh[:-1] + (sh[-1] // 32, 2, 16))
    a = a[..., ::-1, :]
    return np.ascontiguousarray(a.reshape(sh))


def _kc_layout(w):
    sh = w.shape
    w = w.reshape(sh[:-2] + (sh[-2] // 128, 128, sh[-1]))
    return np.ascontiguousarray(np.swapaxes(w, -3, -2))


def host_prepare(inp, lat, n_layers):
    f = np.float32
    L = n_layers
    out = {}
    w_in = np.asarray(inp["w_in"], f)[:L]
    ext = np.concatenate([w_in, _swap32(w_in[..., C_QDA:C_QDA + 384]), _swap32(w_in[..., C_KDA:C_KDA + 384]),
                          _swap32(w_in[..., C_KR:C_KR + 32])], axis=-1)
    out["w_in_r"] = _kc_layout(ext)
    out["w_out_r"] = _kc_layout(np.asarray(inp["w_out"], f)[:L])
    wuq = np.asarray(inp["mla_w_uq"], f)[:L].reshape(L, 256, 6, 96)
    nope = wuq[..., :64].reshape(L, 256, 384)
    rope = wuq[..., 64:].reshape(L, 256, 192)
    out["w_uq_r"] = _kc_layout(np.concatenate([nope, rope, _swap32(rope)], axis=-1))
    wukv = np.asarray(inp["mla_w_ukv"], f)[:L].reshape(L, 128, 6, 128)
    out["w_ukv_r"] = np.ascontiguousarray(np.concatenate([wukv[..., :64].reshape(L, 128, 384), wukv[..., 64:].reshape(L, 128, 384)], axis=-1))
    wa = np.asarray(inp["lru_wa"], f)[:L]
    wi = np.asarray(inp["lru_wi"], f)[:L]
    lw = np.zeros((L, 128, 8, 128), f)
    for d in range(2):
        for c in range(2):
            for ai, w in enumerate((wa, wi)):
                for hb in range(2):
                    lw[:, hb * 64:(hb + 1) * 64, d * 4 + c * 2 + ai, hb * 64:(hb + 1) * 64] = w[:, d, 2 * c + hb]
    out["lru_w_r"] = lw
    w1 = _kc_layout(np.asarray(inp["exp_w1"], f)[:L])
    w3 = _kc_layout(np.asarray(inp["exp_w3"], f)[:L])
    out["w13_r"] = np.ascontiguousarray(np.stack([w1, w3], axis=3))
    w2 = np.asarray(inp["exp_w2"], f)[:L].reshape(L, NE, 2, 128, 8, 128)
    out["w2_r"] = np.ascontiguousarray(w2.transpose(0, 4, 3, 1, 2, 5))
    wm = np.asarray(inp["w_mod"], f)[:L].reshape(L, 8, 128, 12, 512)
    out["w_mod_r"] = np.ascontiguousarray(wm.transpose(0, 3, 2, 1, 4))
    out["b_mod_r"] = np.ascontiguousarray(np.asarray(inp["b_mod"], f)[:L].reshape(L, 48, 128).transpose(2, 0, 1))
    out["router_r"] = _kc_layout(np.asarray(inp["router_w"], f))
    out["router_b_r"] = np.ascontiguousarray(np.broadcast_to(np.asarray(inp["router_b"], f)[None, :], (128, NE)))
    small = np.zeros((128, L, 40), f)

    def chunked(v, n):
        return np.asarray(v, f)[:L].reshape(L, n, 128).transpose(2, 0, 1)

    small[:, :, 0:8] = chunked(inp["ln1_g"], 8)
    small[:, :, 8:16] = chunked(inp["ln1_b"], 8)
    small[:, :, 16:24] = chunked(inp["ln2_g"], 8)
    small[:, :, 24:32] = chunked(inp["ln2_b"], 8)
    small[:, :, 32:34] = chunked(inp["mla_q_norm"], 2)
    small[:, :, 34:35] = chunked(inp["mla_kv_norm"], 1)
    small[:, :, 35:37] = chunked(inp["conv_b"], 2)
    dn = np.asarray(inp["diff_norm"], f)[:L]
    small[:, :, 37] = np.concatenate([dn, dn], axis=1).T
    small[:, :, 38] = RMS_EPS
    small[:, :, 39] = LN_EPS_EFF
    out["small_r"] = small
    small2 = np.zeros((128, L, 16), f)
    cw = np.asarray(inp["conv_w"], f)[:L].reshape(L, 4, 2, 128)
    small2[:, :, 0:8] = cw.transpose(3, 0, 1, 2).reshape(128, L, 8)
    for nm, o in (("lru_ba", 8), ("lru_bi", 12)):
        v = np.asarray(inp[nm], f)[:L].reshape(L, 2, 2, 128)
        small2[:, :, o:o + 4] = v.transpose(3, 0, 1, 2).reshape(128, L, 4)
    out["small2_r"] = small2
    lam = np.asarray(inp["lru_lambda"], f)[:L].reshape(L, 2, 2, 128)
    out["lam_r"] = np.ascontiguousarray(lam.transpose(3, 0, 1, 2).reshape(128, L, 4))
    out["dlam_r"] = np.ascontiguousarray(np.broadcast_to(np.asarray(inp["diff_lambda"], f)[:L][None], (128, L, 4, 32)))
    t = np.arange(lat)
    row = (t // GRID_W).astype(np.float64)
    col = (t % GRID_W).astype(np.float64)
    inv = 10000.0 ** (-np.arange(8, dtype=np.float64) / 8)
    ang = np.concatenate([row[:, None] * inv, col[:, None] * inv], axis=-1)
    ang32 = np.concatenate([row.astype(f)[:, None] * inv.astype(f), col.astype(f)[:, None] * inv.astype(f)], axis=-1).astype(f)
    cos = np.cos(ang32).astype(f)
    sin = np.sin(ang32).astype(f)
    cosT = np.zeros((128, lat), f)
    sinT = np.zeros((128, lat), f)
    for p in range(128):
        j = p % 32
        cosT[p] = cos[:, j % 16]
        sinT[p] = -sin[:, j % 16] if j < 16 else sin[:, j % 16]
    out["cosT"] = cosT
    out["sinT"] = sinT
    out["ident"] = np.eye(128, dtype=f)
    import ml_dtypes
    selm = np.zeros((16, NE, 128), ml_dtypes.bfloat16)
    for e in range(NE):
        selm[e, e, :] = 1.0
    out["selm"] = selm
    return out


def kernel(**inputs):
    return run_kernel(inputs)


def run_kernel(inputs, lat=4096, n_layers=DEPTH, n_cores=8, nb=2, debug=(), stop_after=None):
    shared = host_prepare(inputs, lat, n_layers)
    x = np.asarray(inputs["x"], np.float32)
    ctx = np.asarray(inputs["ctx"], np.float32)
    c = np.asarray(inputs["c"], np.float32)
    c_ctx = np.asarray(inputs["c_ctx"], np.float32)
    nc = build_program(lat=lat, n_layers=n_layers, nb=nb, debug=debug, stop_after=stop_after)
    in_maps = []
    for core in range(n_cores):
        m = dict(shared)
        bs = slice(core * nb, (core + 1) * nb)
        m["x"] = np.ascontiguousarray(x[bs])
        m["ctx"] = np.ascontiguousarray(ctx[bs])
        cT = np.zeros((128, KC, 4), np.float32)
        for j in range(nb):
            cT[:, :, j] = c[core * nb + j].reshape(KC, 128).T
        cT[:, :, 2] = c_ctx.reshape(KC, 128).T
        m["cT"] = cT
        in_maps.append(m)
    res = run_bass_kernel_spmd(nc, in_maps, core_ids=list(range(n_cores)))
    out = np.concatenate([np.asarray(r["out"]) for r in res.results], axis=0)
    if debug:
        return out, res.results
    return out
```

```python
import math
from contextlib import ExitStack

import numpy as np
import concourse.bass as bass
import concourse.mybir as mybir
from concourse.bass_utils import run_bass_kernel_spmd

F32 = mybir.dt.float32
BF16 = mybir.dt.bfloat16
AF = mybir.ActivationFunctionType
ALU = mybir.AluOpType
AX = mybir.AxisListType

D = 1024
KC = 8
DEPTH = 4
CTX = 256
GRID_W = 64
NE = 16
DE = 256
ALPHA = (2 * DEPTH) ** 0.25
LN_EPS_EFF = 1e-5 / (ALPHA * ALPHA)
RMS_EPS = 1e-6
DA_SCALE = 32 ** -0.5
MLA_SCALE = 96 ** -0.5
WIN_COLS = 2880
C_XB, C_GATE, C_QDA, C_KDA, C_VDA, C_CQ, C_CKV, C_KR = 0, 256, 512, 896, 1280, 1664, 1920, 2048
C_QDA_SW, C_KDA_SW, C_KR_SW = 2080, 2464, 2848


class S:
    EPOCH = 16000
    NDMA = 20

    def __init__(self, nc, es):
        self.nc = nc
        self.es = es
        self.eng = {"pe": nc.tensor, "act": nc.scalar, "dve": nc.vector, "pool": nc.gpsimd, "sp": nc.sync}
        self.names = list(self.eng)
        self.esems = {e: [] for e in self.names}
        self.count = {e: 0 for e in self.names}
        self.waited = {e: {e2: 0 for e2 in self.names} for e in self.names}
        self.pe_pending = False
        self.dma_sems = {}
        self.dma_rr = {}
        self.dma_total = {}
        self.dma_waited = {e: {} for e in self.names}
        for q in ("sp", "act", "pool"):
            self.dma_sems[q] = [es.enter_context(nc.semaphore(f"dq_{q}_{i}")) for i in range(self.NDMA)]
            self.dma_rr[q] = 0
            for s_ in self.dma_sems[q]:
                self.dma_total[s_.name] = 0
        self.sem_by_name = {s_.name: s_ for q in self.dma_sems for s_ in self.dma_sems[q]}
        self.bar_sem = es.enter_context(nc.semaphore("barrier"))
        self.bar_count = 0
        self.last_w = {}
        self.readers = {}
        self.last_x = {}
        self.n_ops = 0

    def _esem(self, e, n):
        k = (n - 1) // self.EPOCH
        while len(self.esems[e]) <= k:
            self.esems[e].append(self.es.enter_context(self.nc.semaphore(f"es_{e}_{len(self.esems[e])}")))
        return self.esems[e][k], n - k * self.EPOCH

    def _wait(self, e, tok):
        if tok[0] == "e":
            _, e2, n = tok
            if self.waited[e][e2] >= n:
                return
            assert n <= self.count[e2], f"waiting on unsignalled op of {e2}: {n} > {self.count[e2]}"
            sem, val = self._esem(e2, n)
            self.eng[e].wait_ge(sem, val)
            self.waited[e][e2] = n
        else:
            _, sname, val = tok
            if self.dma_waited[e].get(sname, 0) >= val:
                return
            self.eng[e].wait_ge(self.sem_by_name[sname], val)
            self.dma_waited[e][sname] = val

    def _deps(self, e, reads, writes, psum):
        raw = set()
        oth = set()
        for k in reads:
            t = self.last_w.get(k)
            if t is not None:
                raw.add(t)
        for k in writes:
            t = self.last_w.get(k)
            if t is not None:
                oth.add(t)
            for t in self.readers.get(k, ()):
                oth.add(t)
        for k in psum:
            t = self.last_x.get(k)
            if t is not None:
                oth.add(t)
        for t in raw:
            if t[0] == "e" and t[1] == e and e == "pe":
                continue
            self._wait(e, t)
        for t in oth:
            if t[0] == "e" and t[1] == e:
                continue
            self._wait(e, t)

    def _register(self, tok, reads, writes, psum):
        for k in reads:
            self.readers.setdefault(k, []).append(tok)
        for k in writes:
            self.last_w[k] = tok
            self.readers[k] = []
        for k in psum:
            self.last_x[k] = tok

    def op(self, e, fn, reads=(), writes=(), psum=(), signal=True):
        self._deps(e, reads, writes, psum)
        ins = fn()
        self.n_ops += 1
        if signal:
            self.count[e] += 1
            sem, _ = self._esem(e, self.count[e])
            ins.then_inc(sem, 1)
            tok = ("e", e, self.count[e])
            if e == "pe":
                self.pe_pending = False
        else:
            assert e == "pe"
            tok = ("e", e, self.count[e] + 1)
            self.pe_pending = True
        self._register(tok, reads, writes, psum)
        return tok

    def dma(self, q, out, in_, reads=(), writes=(), after=(), **kw):
        self._deps(q, reads, writes, ())
        for t in after:
            self._wait(q, t)
        i = self.dma_rr[q]
        self.dma_rr[q] = (i + 1) % self.NDMA
        sem = self.dma_sems[q][i]
        tot = self.dma_total[sem.name]
        if tot > 0:
            self._wait(q, ("d", sem.name, tot))
        self.eng[q].dma_start(out=out, in_=in_, **kw).then_inc(sem, 16)
        self.n_ops += 1
        self.dma_total[sem.name] = tot + 16
        tok = ("d", sem.name, tot + 16)
        self._register(tok, reads, writes, ())
        return tok

    def barrier(self):
        assert not self.pe_pending
        for e2 in self.names:
            if e2 != "sp" and self.count[e2] > 0:
                self._wait("sp", ("e", e2, self.count[e2]))
        for name, tot in self.dma_total.items():
            if tot > 0 and not name.startswith("dq_pool"):
                self._wait("sp", ("d", name, tot))
        self.bar_count += 1
        self.eng["sp"].sem_inc(self.bar_sem, 1)
        for e in self.names:
            if e != "sp":
                self.eng[e].wait_ge(self.bar_sem, self.bar_count)
        for e in self.names:
            for e2 in self.names:
                self.waited[e][e2] = self.count[e2]
            for name, tot in self.dma_total.items():
                if not name.startswith("dq_pool"):
                    self.dma_waited[e][name] = tot
        self.last_w.clear()
        self.readers.clear()
        self.last_x.clear()

    def finish(self):
        assert not self.pe_pending
        for e2 in self.names:
            if e2 != "sp" and self.count[e2] > 0:
                self._wait("sp", ("e", e2, self.count[e2]))
        for name, tot in self.dma_total.items():
            if tot > 0:
                self._wait("sp", ("d", name, tot))


class Banks:
    def __init__(self, nc, es):
        self.t = [es.enter_context(nc.psum_tensor(f"psum{i}", [128, 2, 512], F32)) for i in range(4)]
        self.rr = 0

    def bank(self, i):
        return self.t[i // 2][:, i % 2, :]

    def pair(self, i):
        return self.t[i]

    def next(self):
        i = self.rr
        self.rr = (self.rr + 1) % 8
        return i


def bkey(i):
    return ("psb", i)


def build_program(lat=4096, n_layers=DEPTH, nb=2, debug=(), stop_after=None):
    ntok = CTX + lat
    nkb = ntok // 128
    chunks = [(0, CTX, True)] + [(CTX + 512 * i, 512, False) for i in range(lat // 512)]
    XBW = ntok + 6
    nc = bass.Bass("TRN2", target_bir_lowering=False)

    def din(name, shape, dt=F32):
        return nc.dram_tensor(name, list(shape), dt, kind="ExternalInput").ap()

    def dscr(name, shape, dt):
        kind = "ExternalOutput" if name in debug else "Internal"
        return nc.dram_tensor(name, list(shape), dt, kind=kind).ap()

    x_in = din("x", [nb, lat, D])
    ctx_in = din("ctx", [nb, CTX, D])
    cT_in = din("cT", [128, KC, 4])
    wmod_in = din("w_mod_r", [n_layers, 12, 128, KC, 512])
    bmod_in = din("b_mod_r", [128, n_layers, 48])
    win_in = din("w_in_r", [n_layers, 128, KC, WIN_COLS])
    wout_in = din("w_out_r", [n_layers, 128, KC, D])
    wuq_in = din("w_uq_r", [n_layers, 128, 2, 768])
    wukv_in = din("w_ukv_r", [n_layers, 128, 768])
    lruw_in = din("lru_w_r", [n_layers, 128, 8, 128])
    w13_in = din("w13_r", [n_layers, NE, 128, 2, KC, DE])
    w2_in = din("w2_r", [n_layers, 8, 128, NE, 2, 128])
    router_in = din("router_r", [128, KC, NE])
    rb_in = din("router_b_r", [128, NE])
    small_in = din("small_r", [128, n_layers, 40])
    dlam_in = din("dlam_r", [128, n_layers, 4, 32])
    cos_in = din("cosT", [128, lat])
    sin_in = din("sinT", [128, lat])
    ident_in = din("ident", [128, 128])
    selm_in = din("selm", [16, NE, 128], BF16)
    out_d = nc.dram_tensor("out", [nb, lat, D], F32, kind="ExternalOutput").ap()

    win_b = dscr("win_b", [n_layers, 128, KC, WIN_COLS], BF16)
    wout_b = dscr("wout_b", [n_layers, 128, KC, D], BF16)
    wuq_b = dscr("wuq_b", [n_layers, 128, 2, 768], BF16)
    wukv_b = dscr("wukv_b", [n_layers, 128, 768], BF16)
    lruw_b = dscr("lruw_b", [n_layers, 128, 8, 128], BF16)
    w13_b = dscr("w13_b", [n_layers, NE, 128, 2, KC, DE], BF16)
    w2_b = dscr("w2_b", [n_layers, 8, 128, NE, 2, 128], BF16)
    xT_s = dscr("xT_s", [D, ntok], F32)
    xb_s = dscr("xb_s", [256, XBW], F32)
    gg_s = dscr("gg_s", [256, ntok], F32)
    hf_s = dscr("hf_s", [256, ntok], F32)
    hb_s = dscr("hb_s", [256, ntok], F32)
    aT_s = dscr("aT_s", [256, ntok], BF16)
    qda_s = dscr("qda_s", [384, ntok], BF16)
    kda_s = dscr("kda_s", [384, ntok], BF16)
    vda_s = dscr("vda_s", [ntok, 576], BF16)
    qn_s = dscr("qn_s", [384, ntok], BF16)
    qr_s = dscr("qr_s", [192, ntok], BF16)
    kn_s = dscr("kn_s", [384, ntok], BF16)
    kr_s = dscr("kr_s", [32, ntok], BF16)
    vml_s = dscr("vml_s", [ntok, 576], BF16)
    bT_s = dscr("bT_s", [384, ntok], BF16)
    mT_s = dscr("mT_s", [384, ntok], BF16)

    with ExitStack() as es:
        s = S(nc, es)
        pb = Banks(nc, es)

        uniq = [0]

        def sb(stack, name, shape, dt=F32):
            uniq[0] += 1
            return stack.enter_context(nc.sbuf_tensor(f"{name}_{uniq[0]}", list(shape), dt))

        mod = sb(es, "mod", [128, n_layers, 48, 4])
        small = sb(es, "small", [128, n_layers, 40])
        coef = sb(es, "coef", [128, n_layers, 8])
        negl = sb(es, "negl", [128, n_layers])
        gn = sb(es, "gn", [128, n_layers])
        ones_b = sb(es, "ones_b", [128, 128], BF16)
        ones_f = sb(es, "ones_f", [128, 128], F32)
        ident = sb(es, "ident", [128, 128], F32)
        router = sb(es, "router", [128, KC, NE], F32)
        rbias = sb(es, "rbias", [128, NE], F32)
        small2 = sb(es, "small2", [128, n_layers, 24])
        small2_in = din("small2_r", [128, n_layers, 16])
        lam_in = din("lam_r", [128, n_layers, 4])

        conv_tok = {}

        def convert_layer(l_):
            for (dst, src, lead) in ((win_b, win_in, ()), (wout_b, wout_in, ()), (wuq_b, wuq_in, ()), (wukv_b, wukv_in, ()),
                                     (lruw_b, lruw_in, ()), (w13_b, w13_in, (NE,)), (w2_b, w2_in, (8,))):
                toks = []
                for idx in np.ndindex(*lead):
                    full = (l_,) + idx
                    toks.append(s.dma("pool", dst[full], src[full], max_dma_last_dim=4096))
                conv_tok[(dst.name, l_)] = toks

        def wtok(ap_, l_):
            return conv_tok[(ap_.name, l_)]

        with ExitStack() as ps:
            cT = sb(ps, "cT", [128, KC, 4])
            sT = sb(ps, "sT", [128, KC, 4])
            bmod = sb(ps, "bmod", [128, n_layers, 48])
            dlam = sb(ps, "dlam", [128, n_layers, 4, 32])
            dl_p = sb(ps, "dl_p", [128, n_layers, 2, 32])
            dl_s = sb(ps, "dl_s", [128, n_layers, 2])
            dl_e = sb(ps, "dl_e", [128, n_layers, 2])
            lam_t = sb(ps, "lam_t", [128, n_layers, 4])
            lam_e = sb(ps, "lam_e", [128, n_layers, 4])
            zer = sb(ps, "zer", [128, 2, 4])
            wm = [sb(ps, f"wm{i}", [128, KC, 512]) for i in range(2)]

            s.dma("sp", cT[:], cT_in[:, :, :], writes=["cT"])
            s.dma("sp", bmod[:], bmod_in[:, :, :], writes=["bmod"])
            s.dma("sp", small[:], small_in[:, :, :], writes=["small"])
            s.dma("sp", small2[:, :, 0:16], small2_in[:, :, :], writes=["small2"])
            s.op("dve", lambda: nc.vector.tensor_scalar(out=small2[:, :, 16:24], in0=small2[:, :, 8:16], scalar1=-1.0, scalar2=None, op0=ALU.mult),
                 reads=["small2"], writes=["small2"])
            s.dma("sp", dlam[:], dlam_in[:, :, :, :], writes=["dlam"])
            s.dma("sp", lam_t[:], lam_in[:, :, :], writes=["lam_t"])
            s.dma("sp", ident[:], ident_in[:, :], writes=["ident"])
            s.dma("sp", router[:], router_in[:, :, :], writes=["router"])
            s.dma("sp", rbias[:], rb_in[:, :], writes=["rbias"])
            s.op("pool", lambda: nc.gpsimd.memset(ones_b[:], 1.0), writes=["ones_b"])
            s.op("pool", lambda: nc.gpsimd.memset(ones_f[:], 1.0 / D), writes=["ones_f"])
            s.op("pool", lambda: nc.gpsimd.memset(zer[:], 0.0), writes=["zer"])
            for (a, b_) in ((0, 2), (258, 261), (XBW - 1, XBW)):
                s.dma("sp", xb_s.rearrange("(c p) t -> p c t", p=128)[:, :, a:b_], zer[:, :, 0:b_ - a],
                      reads=["zer"], writes=[("xb_pad", a)], allow_slow_non_contiguous=True)

            convert_layer(0)

            s.op("act", lambda: nc.scalar.activation(out=sT[:], in_=cT[:], func=AF.Silu), reads=["cT"], writes=["sT"])
            for l in range(n_layers):
                bk = pb.next()
                bank = pb.bank(bk)
                for g in range(12):
                    w = wm[g % 2]
                    wk = ("wm", g % 2)
                    s.dma("sp", w[:], wmod_in[l, g], writes=[wk])
                    for jj in range(4):
                        j = g * 4 + jj
                        for kc in range(KC):
                            s.op("pe", lambda: nc.tensor.matmul(bank[:, j * 4:j * 4 + 4], w[:, kc, jj * 128:(jj + 1) * 128],
                                                                sT[:, kc, :], start=(kc == 0), stop=(kc == KC - 1)),
                                 reads=[wk, "sT"], writes=[bkey(bk)], psum=[bkey(bk)], signal=(kc == KC - 1))
                s.op("dve", lambda: nc.vector.tensor_tensor(
                    out=mod[:, l, :, :], in0=bank[:, 0:192].rearrange("p (j b) -> p j b", b=4),
                    in1=bmod[:, l, :].unsqueeze(2).to_broadcast([128, 48, 4]), op=ALU.add),
                    reads=[bkey(bk), "bmod"], writes=["mod"], psum=[bkey(bk)])
                for g_ in (1, 4):
                    s.op("dve", lambda: nc.vector.tensor_scalar(out=mod[:, l, g_ * 8:g_ * 8 + 8, :], in0=mod[:, l, g_ * 8:g_ * 8 + 8, :],
                                                                 scalar1=1.0, scalar2=None, op0=ALU.add),
                         reads=["mod"], writes=["mod"])
                for g_ in (2, 5):
                    s.op("dve", lambda: nc.vector.tensor_scalar(out=mod[:, l, g_ * 8:g_ * 8 + 8, :], in0=mod[:, l, g_ * 8:g_ * 8 + 8, :],
                                                                 scalar1=1.0 / ALPHA, scalar2=None, op0=ALU.mult),
                         reads=["mod"], writes=["mod"])
            s.op("dve", lambda: nc.vector.tensor_tensor(out=dl_p[:], in0=dlam[:, :, 0:4:2, :], in1=dlam[:, :, 1:4:2, :], op=ALU.mult),
                 reads=["dlam"], writes=["dl_p"])
            s.op("dve", lambda: nc.vector.tensor_reduce(out=dl_s[:], in_=dl_p[:], axis=AX.X, op=ALU.add),
                 reads=["dl_p"], writes=["dl_s"])
            s.op("act", lambda: nc.scalar.activation(out=dl_e[:], in_=dl_s[:], func=AF.Exp), reads=["dl_s"], writes=["dl_e"])
            s.op("dve", lambda: nc.vector.tensor_tensor(out=negl[:], in0=dl_e[:, :, 1], in1=dl_e[:, :, 0], op=ALU.subtract),
                 reads=["dl_e"], writes=["negl"])
            for l in range(n_layers):
                lam_init = 0.8 - 0.6 * math.exp(-0.3 * l)
                s.op("dve", lambda: nc.vector.tensor_scalar(out=negl[:, l:l + 1], in0=negl[:, l:l + 1], scalar1=-lam_init, scalar2=None, op0=ALU.add),
                     reads=["negl"], writes=["negl"])
                s.op("dve", lambda: nc.vector.tensor_scalar(out=gn[:, l:l + 1], in0=small[:, l, 37:38], scalar1=1.0 - lam_init, scalar2=None, op0=ALU.mult),
                     reads=["small"], writes=["gn"])
            s.op("act", lambda: nc.scalar.activation(out=lam_e[:], in_=lam_t[:], func=AF.Exp, scale=-1.0), reads=["lam_t"], writes=["lam_e"])
            s.op("act", lambda: nc.scalar.activation(out=lam_e[:], in_=lam_e[:], func=AF.Ln, bias=1.0), reads=["lam_e"], writes=["lam_e"])
            s.op("dve", lambda: nc.vector.tensor_scalar(out=coef[:, :, 0:4], in0=lam_e[:], scalar1=-8.0, scalar2=None, op0=ALU.mult),
                 reads=["lam_e"], writes=["coef"])
            s.op("dve", lambda: nc.vector.tensor_scalar(out=coef[:, :, 4:8], in0=lam_e[:], scalar1=-16.0, scalar2=None, op0=ALU.mult),
                 reads=["lam_e"], writes=["coef"])
            s.barrier()

        def fm(scr, nch):
            return scr.rearrange("(c p) t -> p c t", p=128)

        def modcol(l, g, kc, bi):
            return mod[:, l, g * 8 + kc, bi:bi + 1]

        for b in range(nb):
            with ExitStack() as ps:
                xtok = [sb(ps, f"xtok{i}", [128, 4, D]) for i in range(2)]
                xTc = [sb(ps, f"xTc{i}", [128, KC, 512]) for i in range(2)]
                for ci, (pos, N, isctx) in enumerate(chunks):
                    nt = N // 128
                    xt = xtok[ci % 2]
                    xk = ("xtok", ci % 2)
                    xc = xTc[ci % 2]
                    ck = ("xTc", ci % 2)
                    src = ctx_in[b] if isctx else x_in[b, pos - CTX:pos - CTX + N, :]
                    s.dma("sp", xt[:, 0:nt, :], src.rearrange("(t p) d -> p t d", p=128), writes=[xk])
                    for kc in range(KC):
                        bk = pb.next()
                        bank = pb.bank(bk)
                        for t in range(nt):
                            s.op("pe", lambda: nc.tensor.transpose(bank[:, t * 128:(t + 1) * 128], xt[:, t, kc * 128:(kc + 1) * 128], ident[:]),
                                 reads=[xk, "ident"], writes=[bkey(bk)], psum=[bkey(bk)], signal=(t == nt - 1))
                        eng = "act" if kc % 2 == 0 else "dve"
                        if eng == "act":
                            s.op("act", lambda: nc.scalar.copy(out=xc[:, kc, 0:N], in_=bank[:, 0:N]),
                                 reads=[bkey(bk)], writes=[ck], psum=[bkey(bk)])
                        else:
                            s.op("dve", lambda: nc.vector.tensor_copy(out=xc[:, kc, 0:N], in_=bank[:, 0:N]),
                                 reads=[bkey(bk)], writes=[ck], psum=[bkey(bk)])
                    s.dma("sp", fm(xT_s, KC)[:, :, pos:pos + N], xc[:, :, 0:N], reads=[ck], writes=[("xT", ci)])
                s.barrier()

            for l in range(n_layers):
                need_ctx = l < n_layers - 1
                lam_init = 0.8 - 0.6 * math.exp(-0.3 * l)
                conv_next = (lambda l_=l: convert_layer(l_ + 1)) if (b == 0 and l + 1 < n_layers) else None
                layer_phases(nc, s, pb, sb, locals())

        s.finish()
    return nc


def layer_phases(nc, s, pb, sb, env):
    g = env
    b, l, need_ctx = g["b"], g["l"], g["need_ctx"]
    n_layers, lat, ntok, nkb, chunks, XBW = g["n_layers"], g["lat"], g["ntok"], g["nkb"], g["chunks"], g["XBW"]
    mod, small, small2, coef, negl, gn = g["mod"], g["small"], g["small2"], g["coef"], g["negl"], g["gn"]
    ones_b, ones_f, ident, router, rbias = g["ones_b"], g["ones_f"], g["ident"], g["router"], g["rbias"]
    fm, modcol = g["fm"], g["modcol"]
    last = (l == n_layers - 1)

    def mb(isctx):
        return 2 if isctx else b

    with ExitStack() as ps:
        W = sb(ps, "W", [128, KC, WIN_COLS], BF16)
        Wuq = sb(ps, "Wuq", [128, 2, 768], BF16)
        Wukv = sb(ps, "Wukv", [128, 768], BF16)
        xT = [sb(ps, f"xT{i}", [128, KC, 512]) for i in range(2)]
        U = sb(ps, "U", [128, KC, 512], BF16)
        cosT = sb(ps, "cosT_t", [128, 512])
        sinT = sb(ps, "sinT_t", [128, 512])
        tA = [sb(ps, f"tA{i}", [128, 512]) for i in range(2)]
        tB = [sb(ps, f"tB{i}", [128, 512]) for i in range(2)]
        of32 = [sb(ps, f"of32_{i}", [128, 512]) for i in range(4)]
        obf = [sb(ps, f"obf_{i}", [128, 512], BF16) for i in range(4)]
        sq = sb(ps, "sq", [128, 3, 512], BF16)
        rstd = sb(ps, "rstd", [128, 2, 512])
        cqn = sb(ps, "cqn", [128, 2, 512], BF16)
        ckvn = sb(ps, "ckvn", [128, 512], BF16)
        vtok = [sb(ps, f"vtok{i}", [128, 576], BF16) for i in range(2)]
        cnt = {"t": 0, "o": 0, "b": 0, "v": 0}

        s.dma("sp", W[:], g["win_b"][l], writes=["W"], after=g["wtok"](g["win_b"], l))
        s.dma("sp", Wuq[:], g["wuq_b"][l], writes=["Wuq"], after=g["wtok"](g["wuq_b"], l))
        s.dma("sp", Wukv[:], g["wukv_b"][l], writes=["Wukv"], after=g["wtok"](g["wukv_b"], l))
        for i in range(2):
            for c in range(3):
                s.op("pool", lambda: nc.gpsimd.memset(vtok[i][:, c * 192 + 64:c * 192 + 128], 1.0), writes=[("vtok", i)])

        def next_obf():
            i = cnt["b"] % 4
            cnt["b"] += 1
            return obf[i], ("obf", i)

        def next_of32():
            i = cnt["o"] % 4
            cnt["o"] += 1
            return of32[i], ("of32", i)

        def proj(col0, M, N, rhs=None, rk="U", lhs=None, lk="W", nk=KC, prow=0):
            bk = pb.next()
            bank = pb.bank(bk)
            lhs_ = W if lhs is None else lhs
            rhs_ = U if rhs is None else rhs
            for kc in range(nk):
                s.op("pe", lambda: nc.tensor.matmul(bank[prow:prow + M, 0:N], lhs_[:, kc, col0:col0 + M], rhs_[:, kc, 0:N],
                                                    start=(kc == 0), stop=(kc == nk - 1)),
                     reads=[lk, rk], writes=[bkey(bk)], psum=[bkey(bk)], signal=(kc == nk - 1))
            return bk

        def rot(bk_t, bk_sw, M, N, isctx, dst, dk):
            bt = pb.bank(bk_t)
            if isctx:
                s.op("act", lambda: nc.scalar.copy(out=dst[0:M, 0:N], in_=bt[0:M, 0:N]),
                     reads=[bkey(bk_t)], writes=[dk], psum=[bkey(bk_t)])
                return
            bs = pb.bank(bk_sw)
            i = cnt["t"] % 2
            cnt["t"] += 1
            s.op("dve", lambda: nc.vector.tensor_tensor(out=tA[i][0:M, 0:N], in0=bt[0:M, 0:N], in1=cosT[0:M, 0:N], op=ALU.mult),
                 reads=[bkey(bk_t), "cosT"], writes=[("tA", i)], psum=[bkey(bk_t)])
            s.op("dve", lambda: nc.vector.tensor_tensor(out=tB[i][0:M, 0:N], in0=bs[0:M, 0:N], in1=sinT[0:M, 0:N], op=ALU.mult),
                 reads=[bkey(bk_sw), "sinT"], writes=[("tB", i)], psum=[bkey(bk_sw)])
            s.op("pool", lambda: nc.gpsimd.tensor_tensor(out=dst[0:M, 0:N], in0=tA[i][0:M, 0:N], in1=tB[i][0:M, 0:N], op=ALU.add),
                 reads=[("tA", i), ("tB", i)], writes=[dk])

        for ci, (pos, N, isctx) in enumerate(chunks):
            bi = mb(isctx)
            x = xT[ci % 2]
            xk = ("xTl", ci % 2)
            s.dma("sp", x[:, :, 0:N], fm(g["xT_s"], KC)[:, :, pos:pos + N], reads=[("xT", ci)], writes=[xk])
            if not isctx:
                lp = pos - CTX
                s.dma("sp", cosT[:, 0:N], g["cos_in"][:, lp:lp + N], writes=["cosT"])
                s.dma("sp", sinT[:, 0:N], g["sin_in"][:, lp:lp + N], writes=["sinT"])
            for kc in range(KC):
                if kc % 2 == 0:
                    s.op("dve", lambda: nc.vector.tensor_scalar(out=U[:, kc, 0:N], in0=x[:, kc, 0:N], scalar1=modcol(l, 1, kc, bi),
                                                                 scalar2=modcol(l, 0, kc, bi), op0=ALU.mult, op1=ALU.add),
                         reads=[xk, "mod"], writes=["U"])
                else:
                    s.op("act", lambda: nc.scalar.activation(out=U[:, kc, 0:N], in_=x[:, kc, 0:N], func=AF.Identity,
                                                              scale=modcol(l, 1, kc, bi), bias=modcol(l, 0, kc, bi)),
                         reads=[xk, "mod"], writes=["U"])
            for c in range(2):
                bk = proj(C_XB + c * 128, 128, N)
                o, ok = next_of32()
                s.op("act", lambda: nc.scalar.copy(out=o[:, 0:N], in_=pb.bank(bk)[:, 0:N]), reads=[bkey(bk)], writes=[ok], psum=[bkey(bk)])
                off = pos + 2 if isctx else pos + 5
                s.dma("sp", g["xb_s"][c * 128:(c + 1) * 128, off:off + N], o[:, 0:N], reads=[ok], writes=[("xb", c, ci)])
                bk = proj(C_GATE + c * 128, 128, N)
                o, ok = next_of32()
                o2, ok2 = next_of32()
                s.op("act", lambda: nc.scalar.copy(out=o[:, 0:N], in_=pb.bank(bk)[:, 0:N]), reads=[bkey(bk)], writes=[ok], psum=[bkey(bk)])
                s.op("act", lambda: nc.scalar.activation(out=o2[:, 0:N], in_=pb.bank(bk)[:, 0:N], func=AF.Square), reads=[bkey(bk)], writes=[ok2], psum=[bkey(bk)])
                s.op("pool", lambda: nc.gpsimd.tensor_scalar(out=o2[:, 0:N], in0=o2[:, 0:N], scalar1=0.044715, scalar2=1.0, op0=ALU.mult, op1=ALU.add),
                     reads=[ok2], writes=[ok2])
                s.op("pool", lambda: nc.gpsimd.tensor_tensor(out=o2[:, 0:N], in0=o2[:, 0:N], in1=o[:, 0:N], op=ALU.mult), reads=[ok2, ok], writes=[ok2])
                s.op("act", lambda: nc.scalar.activation(out=o2[:, 0:N], in_=o2[:, 0:N], func=AF.Sigmoid, scale=2.0 * math.sqrt(2.0 / math.pi)),
                     reads=[ok2], writes=[ok2])
                s.op("pool", lambda: nc.gpsimd.tensor_tensor(out=o[:, 0:N], in0=o2[:, 0:N], in1=o[:, 0:N], op=ALU.mult), reads=[ok2, ok], writes=[ok])
                s.dma("sp", g["gg_s"][c * 128:(c + 1) * 128, pos:pos + N], o[:, 0:N], reads=[ok], writes=[("gg", c, ci)])
            for (c0, c0sw, scr, nm) in ((C_QDA, C_QDA_SW, g["qda_s"], "qda"), (C_KDA, C_KDA_SW, g["kda_s"], "kda")):
                for c in range(3):
                    bk_t = proj(c0 + c * 128, 128, N)
                    bk_s = None if isctx else proj(c0sw + c * 128, 128, N)
                    o, ok = next_obf()
                    rot(bk_t, bk_s, 128, N, isctx, o, ok)
                    s.dma("sp", scr[c * 128:(c + 1) * 128, pos:pos + N], o[:, 0:N], reads=[ok], writes=[(nm, c, ci)])
            bk_t = proj(C_KR, 32, N)
            bk_s = None if isctx else proj(C_KR_SW, 32, N)
            o, ok = next_obf()
            rot(bk_t, bk_s, 32, N, isctx, o, ok)
            s.dma("sp", g["kr_s"][:, pos:pos + N], o[0:32, 0:N], reads=[ok], writes=[("kr", ci)])
            for t in range(N // 128):
                bk = pb.next()
                bank = pb.bank(bk)
                for kc in range(KC):
                    s.op("pe", lambda: nc.tensor.matmul(bank[:, 0:384], U[:, kc, t * 128:(t + 1) * 128], W[:, kc, C_VDA:C_VDA + 384],
                                                        start=(kc == 0), stop=(kc == KC - 1)),
                         reads=["U", "W"], writes=[bkey(bk)], psum=[bkey(bk)], signal=(kc == KC - 1))
                vi = cnt["v"] % 2
                cnt["v"] += 1
                vt = vtok[vi]
                vv = vt[:, :].rearrange("p (c x) -> p c x", x=192)
                bv = bank[:, 0:384].rearrange("p (c x) -> p c x", x=128)
                s.op("act", lambda: nc.scalar.copy(out=vv[:, :, 0:64], in_=bv[:, :, 0:64]), reads=[bkey(bk)], writes=[("vtok", vi)], psum=[bkey(bk)])
                s.op("dve", lambda: nc.vector.tensor_copy(out=vv[:, :, 128:192], in_=bv[:, :, 64:128]), reads=[bkey(bk)], writes=[("vtok", vi)], psum=[bkey(bk)])
                s.dma("sp", g["vda_s"][pos + t * 128:pos + (t + 1) * 128, :], vt[:, :], reads=[("vtok", vi)], writes=[("vda", ci, t)])
            bk_cq = [proj(C_CQ + c * 128, 128, N) for c in range(2)]
            bk_ckv = proj(C_CKV, 128, N)
            for c in range(2):
                s.op("act", lambda: nc.scalar.activation(out=sq[:, c, 0:N], in_=pb.bank(bk_cq[c])[:, 0:N], func=AF.Square),
                     reads=[bkey(bk_cq[c])], writes=[("sq", c)], psum=[bkey(bk_cq[c])])
            s.op("act", lambda: nc.scalar.activation(out=sq[:, 2, 0:N], in_=pb.bank(bk_ckv)[:, 0:N], func=AF.Square),
                 reads=[bkey(bk_ckv)], writes=[("sq", 2)], psum=[bkey(bk_ckv)])
            bk_ss = pb.next()
            for c in range(2):
                s.op("pe", lambda: nc.tensor.matmul(pb.bank(bk_ss)[:, 0:N], ones_b[:, :], sq[:, c, 0:N], start=(c == 0), stop=(c == 1)),
                     reads=["ones_b", ("sq", c)], writes=[bkey(bk_ss)], psum=[bkey(bk_ss)], signal=(c == 1))
            bk_s2 = pb.next()
            s.op("pe", lambda: nc.tensor.matmul(pb.bank(bk_s2)[:, 0:N], ones_b[:, :], sq[:, 2, 0:N], start=True, stop=True),
                 reads=["ones_b", ("sq", 2)], writes=[bkey(bk_s2)], psum=[bkey(bk_s2)])
            for (bk_, j, dim) in ((bk_ss, 0, 256.0), (bk_s2, 1, 128.0)):
                s.op("act", lambda: nc.scalar.activation(out=rstd[:, j, 0:N], in_=pb.bank(bk_)[:, 0:N], func=AF.Ln, scale=1.0 / dim, bias=small[:, l, 38:39]),
                     reads=[bkey(bk_), "small"], writes=[("rstd", j)], psum=[bkey(bk_)])
                s.op("act", lambda: nc.scalar.activation(out=rstd[:, j, 0:N], in_=rstd[:, j, 0:N], func=AF.Exp, scale=-0.5),
                     reads=[("rstd", j)], writes=[("rstd", j)])
            for c in range(2):
                s.op("dve", lambda: nc.vector.scalar_tensor_tensor(out=cqn[:, c, 0:N], in0=pb.bank(bk_cq[c])[:, 0:N], scalar=small[:, l, 32 + c:33 + c],
                                                                    in1=rstd[:, 0, 0:N], op0=ALU.mult, op1=ALU.mult),
                     reads=[bkey(bk_cq[c]), "small", ("rstd", 0)], writes=["cqn"], psum=[bkey(bk_cq[c])])
            s.op("dve", lambda: nc.vector.scalar_tensor_tensor(out=ckvn[:, 0:N], in0=pb.bank(bk_ckv)[:, 0:N], scalar=small[:, l, 34:35],
                                                                in1=rstd[:, 1, 0:N], op0=ALU.mult, op1=ALU.mult),
                 reads=[bkey(bk_ckv), "small", ("rstd", 1)], writes=["ckvn"], psum=[bkey(bk_ckv)])
            for c in range(3):
                bk = proj(c * 128, 128, N, rhs=cqn, rk="cqn", lhs=Wuq, lk="Wuq", nk=2)
                o, ok = next_obf()
                s.op("act", lambda: nc.scalar.copy(out=o[:, 0:N], in_=pb.bank(bk)[:, 0:N]), reads=[bkey(bk)], writes=[ok], psum=[bkey(bk)])
                s.dma("sp", g["qn_s"][c * 128:(c + 1) * 128, pos:pos + N], o[:, 0:N], reads=[ok], writes=[("qn", c, ci)])
            for (r0, M) in ((0, 128), (128, 64)):
                bk_t = proj(384 + r0, M, N, rhs=cqn, rk="cqn", lhs=Wuq, lk="Wuq", nk=2)
                bk_s = None if isctx else proj(576 + r0, M, N, rhs=cqn, rk="cqn", lhs=Wuq, lk="Wuq", nk=2)
                o, ok = next_obf()
                rot(bk_t, bk_s, M, N, isctx, o, ok)
                s.dma("sp", g["qr_s"][r0:r0 + M, pos:pos + N], o[0:M, 0:N], reads=[ok], writes=[("qr", r0, ci)])
            for c in range(3):
                bk = pb.next()
                s.op("pe", lambda: nc.tensor.matmul(pb.bank(bk)[:, 0:N], Wukv[:, c * 128:(c + 1) * 128], ckvn[:, 0:N], start=True, stop=True),
                     reads=["Wukv", "ckvn"], writes=[bkey(bk)], psum=[bkey(bk)])
                o, ok = next_obf()
                s.op("act", lambda: nc.scalar.copy(out=o[:, 0:N], in_=pb.bank(bk)[:, 0:N]), reads=[bkey(bk)], writes=[ok], psum=[bkey(bk)])
                s.dma("sp", g["kn_s"][c * 128:(c + 1) * 128, pos:pos + N], o[:, 0:N], reads=[ok], writes=[("kn", c, ci)])
            for t in range(N // 128):
                bk = pb.next()
                bank = pb.bank(bk)
                s.op("pe", lambda: nc.tensor.matmul(bank[:, 0:384], ckvn[:, t * 128:(t + 1) * 128], Wukv[:, 384:768], start=True, stop=True),
                     reads=["ckvn", "Wukv"], writes=[bkey(bk)], psum=[bkey(bk)])
                vi = cnt["v"] % 2
                cnt["v"] += 1
                vt = vtok[vi]
                vv = vt[:, :].rearrange("p (c x) -> p c x", x=192)
                bv = bank[:, 0:384].rearrange("p (c x) -> p c x", x=128)
                s.op("act", lambda: nc.scalar.copy(out=vv[:, :, 0:64], in_=bv[:, :, 0:64]), reads=[bkey(bk)], writes=[("vtok", vi)], psum=[bkey(bk)])
                s.op("dve", lambda: nc.vector.tensor_copy(out=vv[:, :, 128:192], in_=bv[:, :, 64:128]), reads=[bkey(bk)], writes=[("vtok", vi)], psum=[bkey(bk)])
                s.dma("sp", g["vml_s"][pos + t * 128:pos + (t + 1) * 128, :], vt[:, :], reads=[("vtok", vi)], writes=[("vml", ci, t)])
        s.barrier()

    if g.get("stop_after") == "P1":
        return
    p2_lru(nc, s, pb, sb, g)
    if g.get("stop_after") == "P2":
        return
    p34_attn(nc, s, pb, sb, g, da=True)
    p34_attn(nc, s, pb, sb, g, da=False)
    if g.get("stop_after") == "P4":
        return
    p5_ffn(nc, s, pb, sb, g)


def p2_lru(nc, s, pb, sb, g):
    b, l = g["b"], g["l"]
    chunks, ntok, XBW = g["chunks"], g["ntok"], g["XBW"]
    small, small2, coef = g["small"], g["small2"], g["coef"]
    fm = g["fm"]
    hscr = [g["hf_s"], g["hb_s"]]
    with ExitStack() as ps:
        LW = sb(ps, "LW", [128, 8, 128], BF16)
        s.dma("sp", LW[:], g["lruw_b"][l], writes=["LW"], after=g["wtok"](g["lruw_b"], l))
        T = []
        for d in range(2):
            T.append(dict(
                X=[sb(ps, f"X{d}{i}", [128, 2, 515]) for i in range(2)],
                Y=[sb(ps, f"Y{d}{i}", [128, 2, 512]) for i in range(2)], Yb=[sb(ps, f"Yb{d}{i}", [128, 2, 512], BF16) for i in range(2)],
                R=[sb(ps, f"R{d}{i}", [128, 2, 512]) for i in range(2)], I=[sb(ps, f"I{d}{i}", [128, 2, 512]) for i in range(2)],
                A=[sb(ps, f"A{d}{i}", [128, 2, 512]) for i in range(2)], A2=[sb(ps, f"A2{d}{i}", [128, 2, 512]) for i in range(2)],
                Uu=[sb(ps, f"Uu{d}{i}", [128, 2, 512]) for i in range(2)],
                H=[sb(ps, f"H{d}{i}", [128, 2, 512]) for i in range(2)]))
        HF = [sb(ps, f"HF{i}", [128, 2, 512]) for i in range(2)]
        HB = [sb(ps, f"HB{i}", [128, 2, 512]) for i in range(2)]
        GG = [sb(ps, f"GG{i}", [128, 2, 512]) for i in range(2)]
        OB = [sb(ps, f"OB{i}", [128, 2, 512], BF16) for i in range(2)]
        nch = len(chunks)
        order = [list(range(nch)), [0] + list(range(nch - 1, 0, -1))]
        prev = [None, None]

        def step_all(step):
            info = []
            for dd in range(2):
                ci = order[dd][step]
                pos, N, isctx = chunks[ci]
                t = T[dd]
                xi = step % 2
                x = t["X"][xi]
                xk = ("X", dd, xi)
                off = 0 if isctx else pos + 3
                s.dma("sp", x[:, :, 0:N + 3], fm(g["xb_s"], 2)[:, :, off:off + N + 3], writes=[xk])
                info.append((dd, ci, pos, N, t, xi, x, xk))
            chains = [(i_, c) for i_ in info for c in range(2)]
            for (dd, ci, pos, N, t, xi, x, xk), c in chains:
                Y = t["Y"][xi]
                s.op("pool", lambda: nc.gpsimd.tensor_scalar(out=Y[:, c, 0:N], in0=x[:, c, 0:N], scalar1=small2[:, l, c:c + 1],
                                                              scalar2=small[:, l, 35 + c:36 + c], op0=ALU.mult, op1=ALU.add),
                     reads=[xk, "small", "small2"], writes=[("Y", dd, xi, c)])
            for k in range(1, 4):
                for (dd, ci, pos, N, t, xi, x, xk), c in chains:
                    Y = t["Y"][xi]
                    s.op("dve", lambda: nc.vector.scalar_tensor_tensor(out=Y[:, c, 0:N], in0=x[:, c, k:k + N], scalar=small2[:, l, 2 * k + c:2 * k + c + 1],
                                                                        in1=Y[:, c, 0:N], op0=ALU.mult, op1=ALU.add),
                         reads=[xk, "small2", ("Y", dd, xi, c)], writes=[("Y", dd, xi, c)])
            for (dd, ci, pos, N, t, xi, x, xk), c in chains:
                Y, Yb = t["Y"][xi], t["Yb"][xi]
                s.op("act", lambda: nc.scalar.copy(out=Yb[:, c, 0:N], in_=Y[:, c, 0:N]), reads=[("Y", dd, xi, c)], writes=[("Yb", dd, xi, c)])
            bkm = {}
            for (dd, ci, pos, N, t, xi, x, xk), c in chains:
                Yb = t["Yb"][xi]
                for ai in range(2):
                    bk = pb.next()
                    bkm[(dd, c, ai)] = bk
                    s.op("pe", lambda: nc.tensor.matmul(pb.bank(bk)[:, 0:N], LW[:, dd * 4 + c * 2 + ai, :], Yb[:, c, 0:N], start=True, stop=True),
                         reads=["LW", ("Yb", dd, xi, c)], writes=[bkey(bk)], psum=[bkey(bk)])
            for (dd, ci, pos, N, t, xi, x, xk), c in chains:
                for (nm, ai, bo) in (("R", 0, 8), ("I", 1, 12)):
                    dst = t[nm][xi]
                    bk_ = bkm[(dd, c, ai)]
                    s.op("act", lambda: nc.scalar.activation(out=dst[:, c, 0:N], in_=pb.bank(bk_)[:, 0:N], func=AF.Sigmoid,
                                                              bias=small2[:, l, bo + dd * 2 + c:bo + 1 + dd * 2 + c]),
                         reads=[bkey(bk_), "small2"], writes=[(nm, dd, xi, c)], psum=[bkey(bk_)])
            for (dd, ci, pos, N, t, xi, x, xk), c in chains:
                R, A, A2 = t["R"][xi], t["A"][xi], t["A2"][xi]
                s.op("act", lambda: nc.scalar.activation(out=A[:, c, 0:N], in_=R[:, c, 0:N], func=AF.Exp, scale=coef[:, l, dd * 2 + c:dd * 2 + c + 1]),
                     reads=[("R", dd, xi, c), "coef"], writes=[("A", dd, xi, c)])
                s.op("act", lambda: nc.scalar.activation(out=A2[:, c, 0:N], in_=R[:, c, 0:N], func=AF.Exp, scale=coef[:, l, 4 + dd * 2 + c:5 + dd * 2 + c]),
                     reads=[("R", dd, xi, c), "coef"], writes=[("A2", dd, xi, c)])
            for (dd, ci, pos, N, t, xi, x, xk), c in chains:
                A2 = t["A2"][xi]
                s.op("act", lambda: nc.scalar.activation(out=A2[:, c, 0:N], in_=A2[:, c, 0:N], func=AF.Ln, scale=-1.0, bias=1.0),
                     reads=[("A2", dd, xi, c)], writes=[("A2", dd, xi, c)])
            for (dd, ci, pos, N, t, xi, x, xk), c in chains:
                A2, I, Y = t["A2"][xi], t["I"][xi], t["Y"][xi]
                s.op("act", lambda: nc.scalar.activation(out=A2[:, c, 0:N], in_=A2[:, c, 0:N], func=AF.Exp, scale=0.5),
                     reads=[("A2", dd, xi, c)], writes=[("A2", dd, xi, c)])
                s.op("pool", lambda: nc.gpsimd.tensor_tensor(out=I[:, c, 0:N], in0=I[:, c, 0:N], in1=Y[:, c, 0:N], op=ALU.mult),
                     reads=[("I", dd, xi, c), ("Y", dd, xi, c)], writes=[("I", dd, xi, c)])
            for (dd, ci, pos, N, t, xi, x, xk), c in chains:
                A2, I, Uu = t["A2"][xi], t["I"][xi], t["Uu"][xi]
                s.op("pool", lambda: nc.gpsimd.tensor_tensor(out=Uu[:, c, 0:N], in0=I[:, c, 0:N], in1=A2[:, c, 0:N], op=ALU.mult),
                     reads=[("I", dd, xi, c), ("A2", dd, xi, c)], writes=[("Uu", dd, xi, c)])
            hi = step % 2
            for (dd, ci, pos, N, t, xi, x, xk), c in chains:
                A, Uu = t["A"][xi], t["Uu"][xi]
                h = t["H"][hi]
                hk = ("H", dd, hi, c)
                if prev[dd] is None:
                    init = 0.0
                    rk = []
                else:
                    phi, pN = prev[dd]
                    init = t["H"][phi][:, c, pN - 1:pN] if dd == 0 else t["H"][phi][:, c, 0:1]
                    rk = [("H", dd, phi, c)]
                if dd == 0:
                    s.op("dve", lambda: nc.vector.tensor_tensor_scan(out=h[:, c, 0:N], data0=A[:, c, 0:N], data1=Uu[:, c, 0:N], initial=init,
                                                                      op0=ALU.mult, op1=ALU.add),
                         reads=[("A", dd, xi, c), ("Uu", dd, xi, c)] + rk, writes=[hk])
                else:
                    s.op("dve", lambda: nc.vector.tensor_tensor_scan(out=h[:, c, 0:N][:, ::-1], data0=A[:, c, 0:N][:, ::-1], data1=Uu[:, c, 0:N][:, ::-1],
                                                                      initial=init, op0=ALU.mult, op1=ALU.add),
                         reads=[("A", dd, xi, c), ("Uu", dd, xi, c)] + rk, writes=[hk])
            for (dd, ci, pos, N, t, xi, x, xk) in info:
                s.dma("sp", fm(hscr[dd], 2)[:, :, pos:pos + N], t["H"][hi][:, :, 0:N], reads=[("H", dd, hi, 0), ("H", dd, hi, 1)], writes=[("hs", dd, ci)])
                prev[dd] = (hi, N)

        for step in range(nch):
            step_all(step)
        for ci, (pos, N, isctx) in enumerate(chunks):
            j = ci % 2
            s.dma("sp", HF[j][:, :, 0:N], fm(g["hf_s"], 2)[:, :, pos:pos + N], reads=[("hs", 0, ci)], writes=[("HF", j)])
            s.dma("sp", HB[j][:, :, 0:N], fm(g["hb_s"], 2)[:, :, pos:pos + N], reads=[("hs", 1, ci)], writes=[("HB", j)])
            s.dma("sp", GG[j][:, :, 0:N], fm(g["gg_s"], 2)[:, :, pos:pos + N], writes=[("GG", j)])
            e1 = "dve" if j == 0 else "pool"
            eng1 = nc.vector if j == 0 else nc.gpsimd
            s.op(e1, lambda: eng1.tensor_tensor(out=HF[j][:, :, 0:N], in0=HF[j][:, :, 0:N], in1=HB[j][:, :, 0:N], op=ALU.add),
                 reads=[("HF", j), ("HB", j)], writes=[("HF", j)])
            s.op(e1, lambda: eng1.tensor_tensor(out=OB[j][:, :, 0:N], in0=HF[j][:, :, 0:N], in1=GG[j][:, :, 0:N], op=ALU.mult),
                 reads=[("HF", j), ("GG", j)], writes=[("OB", j)])
            s.dma("sp", fm(g["aT_s"], 2)[:, :, pos:pos + N], OB[j][:, :, 0:N], reads=[("OB", j)], writes=[("aT", ci)])
        s.barrier()


def p34_attn(nc, s, pb, sb, g, da):
    b, l, need_ctx = g["b"], g["l"], g["need_ctx"]
    chunks, ntok, nkb = g["chunks"], g["ntok"], g["nkb"]
    negl, gn, small, ones_b = g["negl"], g["gn"], g["small"], g["ones_b"]
    fm = g["fm"]
    with ExitStack() as ps:
        if da:
            K = sb(ps, "K", [128, 3, ntok], BF16)
            Q = [sb(ps, f"Q{i}", [128, 3, 4, 512], BF16) for i in range(2)]
        else:
            K = sb(ps, "K", [96, 6, ntok], BF16)
            Q = [sb(ps, f"Q{i}", [96, 6, 512], BF16) for i in range(2)]
        VA = sb(ps, "VA", [128, nkb, 576], BF16)
        PT = [sb(ps, f"PT{i}", [128, 2, 512], BF16) for i in range(2)]
        R1 = sb(ps, "R1", [128, 512])
        R2 = sb(ps, "R2", [128, 512])
        Aa = sb(ps, "Aa", [128, 512])
        Bb = sb(ps, "Bb", [128, 512])
        Dd = sb(ps, "Dd", [128, 512])
        Dq = sb(ps, "Dq", [128, 512], BF16)
        Rs = sb(ps, "Rs", [128, 512])
        OT = [sb(ps, f"OT{i}", [128, 512], BF16) for i in range(2)]
        bnds = sorted(set([0, 2] + list(range(10, nkb, 8)) + [nkb]))
        kv_pieces = [(bnds[i_], bnds[i_ + 1]) for i_ in range(len(bnds) - 1)]

        def kvp(kb):
            for p_, (b0, b1) in enumerate(kv_pieces):
                if b0 <= kb < b1:
                    return p_
            raise AssertionError(kb)

        if da:
            for i in range(2):
                s.op("pool", lambda: nc.gpsimd.memset(Q[i][:], 0.0), writes=[("Q", i)])
            if g.get("conv_next") is not None:
                g["conv_next"]()
            for p_, (b0, b1) in enumerate(kv_pieces):
                s.dma("sp", K[:, :, b0 * 128:b1 * 128], fm(g["kda_s"], 3)[:, :, b0 * 128:b1 * 128], writes=[("K", p_, "n")])
                s.dma("sp", VA[:, b0:b1, :], g["vda_s"].rearrange("(k p) d -> p k d", p=128)[:, b0:b1, :], writes=[("VA", p_)])
        else:
            for p_, (b0, b1) in enumerate(kv_pieces):
                s.dma("sp", K[0:64, :, b0 * 128:b1 * 128], g["kn_s"].rearrange("(h p) t -> p h t", p=64)[:, :, b0 * 128:b1 * 128],
                      writes=[("K", p_, "n")])
                for h in range(6):
                    s.dma("sp", K[64:96, h, b0 * 128:b1 * 128], g["kr_s"][:, b0 * 128:b1 * 128], writes=[("K", p_, "r", h)])
                s.dma("sp", VA[:, b0:b1, :], g["vml_s"].rearrange("(k p) d -> p k d", p=128)[:, b0:b1, :], writes=[("VA", p_)])
        act_chunks = [(ci, pos, N, isctx) for ci, (pos, N, isctx) in enumerate(chunks) if not (isctx and not need_ctx)]
        nmaps = 2 if da else 1
        units = []
        o_rr = 0
        ot_rr = 0
        for k_, (ci, pos, N, isctx) in enumerate(act_chunks):
            kbs = [0, 1] if isctx else list(range(nkb))
            for c in range(3):
                oti = ot_rr % 2
                ot_rr += 1
                for hh in range(2):
                    obk = []
                    for m in range(nmaps):
                        ob = 4 + (o_rr % 3)
                        o_rr += 1
                        obk.append(ob)
                        npair = len(kbs) // 2
                        for pi in range(npair):
                            units.append(dict(k=k_, ci=ci, pos=pos, N=N, c=c, hh=hh, m=m, ob=ob, obk=list(obk), oti=oti,
                                              kb=(kbs[2 * pi], kbs[2 * pi + 1]), first=(pi == 0), last=(pi == npair - 1),
                                              head_done=(pi == npair - 1 and m == nmaps - 1), pair_done=(pi == npair - 1 and m == nmaps - 1 and hh == 1)))

        def load_q(k_):
            ci, pos, N, isctx = act_chunks[k_]
            q = Q[k_ % 2]
            qk = ("Q", k_ % 2)
            if da:
                for j in range(4):
                    s.dma("sp", q[j * 32:(j + 1) * 32, :, j, 0:N], fm(g["qda_s"], 3)[j * 32:(j + 1) * 32, :, pos:pos + N], writes=[qk])
            else:
                s.dma("sp", q[0:64, :, 0:N], g["qn_s"].rearrange("(h p) t -> p h t", p=64)[:, :, pos:pos + N], writes=[qk])
                s.dma("sp", q[64:96, :, 0:N], g["qr_s"].rearrange("(h p) t -> p h t", p=32)[:, :, pos:pos + N], writes=[qk])

        def emit_qk(i):
            u = units[i]
            N, c, hh, m = u["N"], u["c"], u["hh"], u["m"]
            q = Q[u["k"] % 2]
            qk = ("Q", u["k"] % 2)
            sp_i = i % 2
            Sp = pb.pair(sp_i)
            sk = [bkey(2 * sp_i), bkey(2 * sp_i + 1)]
            h = 2 * c + hh
            for j in range(2):
                kb = u["kb"][j]
                if da:
                    s.op("pe", lambda: nc.tensor.matmul(Sp[:, j, 0:N], K[:, c, kb * 128:(kb + 1) * 128],
                                                        q[:, c, hh * 2 + m, 0:N], start=True, stop=True),
                         reads=[("K", kvp(kb), "n"), qk], writes=[sk[j]], psum=[sk[j]], signal=(j == 1))
                else:
                    s.op("pe", lambda: nc.tensor.matmul(Sp[:, j, 0:N], K[0:96, h, kb * 128:(kb + 1) * 128],
                                                        q[0:96, h, 0:N], start=True, stop=True),
                         reads=[("K", kvp(kb), "n"), ("K", kvp(kb), "r", h), qk], writes=[sk[j]], psum=[sk[j]], signal=(j == 1))

        def emit_exp(i):
            u = units[i]
            N = u["N"]
            sp_i = i % 2
            Sp = pb.pair(sp_i)
            sk = [bkey(2 * sp_i), bkey(2 * sp_i + 1)]
            s.op("act", lambda: nc.scalar.activation(out=PT[sp_i][:, :, 0:N], in_=Sp[:, :, 0:N], func=AF.Exp,
                                                      scale=(DA_SCALE if da else MLA_SCALE)),
                 reads=sk, writes=[("PT", sp_i)], psum=sk)

        def emit_pv(i):
            u = units[i]
            N, c, hh = u["N"], u["c"], u["hh"]
            sp_i = i % 2
            ob = u["ob"]
            vcol = c * 192 + hh * 64
            for j in range(2):
                kb = u["kb"][j]
                s.op("pe", lambda: nc.tensor.matmul(pb.bank(ob)[:, 0:N], VA[:, kb, vcol:vcol + 128], PT[sp_i][:, j, 0:N],
                                                    start=(u["first"] and j == 0), stop=(u["last"] and j == 1)),
                     reads=[("VA", kvp(kb)), ("PT", sp_i)], writes=[bkey(ob)], psum=[bkey(ob)], signal=(j == 1))

        def epi_a(u):
            N, hh = u["N"], u["hh"]
            nb_ = hh * 64
            nsl = slice(nb_, nb_ + 64)
            dsl = slice(64 - nb_, 128 - nb_)
            obk = u["obk"]
            ot = OT[u["oti"]]
            otk = ("OT", u["oti"])
            O1 = pb.bank(obk[0])
            s.op("dve", lambda: nc.vector.reciprocal(out=R1[nsl, 0:N], in_=O1[dsl, 0:N]), reads=[bkey(obk[0])], writes=["R1"], psum=[bkey(obk[0])])
            if not da:
                s.op("dve", lambda: nc.vector.tensor_tensor(out=ot[nsl, 0:N], in0=O1[nsl, 0:N], in1=R1[nsl, 0:N], op=ALU.mult),
                     reads=[bkey(obk[0]), "R1"], writes=[otk], psum=[bkey(obk[0])])
                return
            O2 = pb.bank(obk[1])
            s.op("dve", lambda: nc.vector.reciprocal(out=R2[nsl, 0:N], in_=O2[dsl, 0:N]), reads=[bkey(obk[1])], writes=["R2"], psum=[bkey(obk[1])])
            s.op("dve", lambda: nc.vector.tensor_tensor(out=Aa[nsl, 0:N], in0=O1[nsl, 0:N], in1=R1[nsl, 0:N], op=ALU.mult),
                 reads=[bkey(obk[0]), "R1"], writes=["Aa"], psum=[bkey(obk[0])])
            s.op("dve", lambda: nc.vector.tensor_tensor(out=Bb[nsl, 0:N], in0=O2[nsl, 0:N], in1=R2[nsl, 0:N], op=ALU.mult),
                 reads=[bkey(obk[1]), "R2"], writes=["Bb"], psum=[bkey(obk[1])])
            s.op("dve", lambda: nc.vector.scalar_tensor_tensor(out=Dd[nsl, 0:N], in0=Bb[nsl, 0:N], scalar=negl[nsl, l:l + 1], in1=Aa[nsl, 0:N],
                                                                op0=ALU.mult, op1=ALU.add),
                 reads=["Aa", "Bb", "negl"], writes=[("Dd", hh)])
            s.op("dve", lambda: nc.vector.tensor_tensor(out=Dq[nsl, 0:N], in0=Dd[nsl, 0:N], in1=Dd[nsl, 0:N], op=ALU.mult),
                 reads=[("Dd", hh)], writes=[("Dq", hh)])

        def epi_b(u):
            N, hh = u["N"], u["hh"]
            nb_ = hh * 64
            nsl = slice(nb_, nb_ + 64)
            ot = OT[u["oti"]]
            otk = ("OT", u["oti"])
            rb = pb.bank(7)
            s.op("pe", lambda: nc.tensor.matmul(rb[nsl, 0:N], ones_b[nsl, 0:64], Dq[nsl, 0:N], start=True, stop=True, tile_position=(nb_, nb_)),
                 reads=["ones_b", ("Dq", hh)], writes=[bkey(7)], psum=[bkey(7)])
            s.op("act", lambda: nc.scalar.activation(out=Rs[nsl, 0:N], in_=rb[nsl, 0:N], func=AF.Ln, scale=1.0 / 64.0, bias=small[nsl, l, 38:39]),
                 reads=[bkey(7), "small"], writes=[("Rs", hh)], psum=[bkey(7)])
            s.op("act", lambda: nc.scalar.activation(out=Rs[nsl, 0:N], in_=Rs[nsl, 0:N], func=AF.Exp, scale=-0.5), reads=[("Rs", hh)], writes=[("Rs", hh)])
            s.op("dve", lambda: nc.vector.scalar_tensor_tensor(out=ot[nsl, 0:N], in0=Dd[nsl, 0:N], scalar=gn[nsl, l:l + 1], in1=Rs[nsl, 0:N],
                                                                op0=ALU.mult, op1=ALU.mult),
                 reads=[("Dd", hh), "gn", ("Rs", hh)], writes=[otk])

        def store(u):
            scr = g["bT_s"] if da else g["mT_s"]
            c, pos, N = u["c"], u["pos"], u["N"]
            s.dma("sp", scr[c * 128:(c + 1) * 128, pos:pos + N], OT[u["oti"]][:, 0:N], reads=[("OT", u["oti"])], writes=[("bm", da, c, u["ci"])])

        deferred = []
        nU = len(units)
        load_q(0)
        emit_qk(0)
        DEFER = 12
        for i in range(nU):
            u = units[i]
            if u["first"] and u["c"] == 0 and u["hh"] == 0 and u["m"] == 0 and u["k"] + 1 < len(act_chunks):
                load_q(u["k"] + 1)
            if i + 1 < nU:
                emit_qk(i + 1)
            emit_exp(i)
            emit_pv(i)
            if u["head_done"]:
                while deferred:
                    _, kind, uu = deferred.pop(0)
                    (epi_b if kind == "b" else store)(uu)
                epi_a(u)
                if da:
                    deferred.append((i + DEFER, "b", u))
                    if u["pair_done"]:
                        deferred.append((i + DEFER, "s", u))
                elif u["pair_done"]:
                    store(u)
            while deferred and deferred[0][0] <= i:
                _, kind, uu = deferred.pop(0)
                (epi_b if kind == "b" else store)(uu)
        for _, kind, uu in deferred:
            (epi_b if kind == "b" else store)(uu)
        s.barrier()


def p5_ffn(nc, s, pb, sb, g):
    b, l, need_ctx = g["b"], g["l"], g["need_ctx"]
    n_layers, chunks, ntok = g["n_layers"], g["chunks"], g["ntok"]
    mod, small, ones_f, ident, router, rbias = g["mod"], g["small"], g["ones_f"], g["ident"], g["router"], g["rbias"]
    fm, modcol = g["fm"], g["modcol"]
    last = (l == n_layers - 1)
    F32R = mybir.dt.float32r
    with ExitStack() as ps:
        Wo = sb(ps, "Wo", [128, KC, D], BF16)
        MIX = sb(ps, "MIX", [128, KC, 512], BF16)
        XR = [sb(ps, f"Xr{i}", [128, KC, 512]) for i in range(2)]
        SQ = [sb(ps, f"SQ{i}", [128, 512], F32R) for i in range(2)]
        ones_r = sb(ps, "ones_r", [128, 128], F32R)
        s.op("act", lambda: nc.scalar.copy(out=ones_r[:], in_=ones_f[:]), reads=["ones_f"], writes=["ones_r"])
        ST = [sb(ps, f"ST{i}", [128, 512]) for i in range(4)]
        V32 = sb(ps, "V32", [128, KC, 512])
        Vb = sb(ps, "Vb", [128, KC, 512], BF16)
        GA = sb(ps, "GA", [128, NE, 2, 512], BF16)
        W13 = [sb(ps, f"W13_{i}", [128, 2, KC, DE], BF16) for i in range(3)]
        W2 = [sb(ps, f"W2_{i}", [128, NE, 2, 128], BF16) for i in range(3)]
        SC = sb(ps, "SC", [128, 4, NE])
        SEL = sb(ps, "SEL", [128, 4, NE])
        SEL2 = sb(ps, "SEL2", [128, 4, NE])
        EQ = sb(ps, "EQ", [128, 4, NE])
        M1 = sb(ps, "M1", [128, 16])
        M2 = sb(ps, "M2", [128, 16])
        GS = sb(ps, "GS", [128, 16])
        GM = sb(ps, "GM", [128, 4])
        ING = sb(ps, "ING", [128, 16])
        GT = sb(ps, "GT", [128, 4, NE])
        DEN = sb(ps, "DEN", [128, 4])
        GTT = sb(ps, "GTT", [16, 512], BF16)
        SELM = sb(ps, "SELM", [16, NE, 128], BF16)
        SIL = [sb(ps, f"SIL{i}", [128, 512]) for i in range(2)]
        HH = [sb(ps, f"HH{i}", [128, 512]) for i in range(2)]
        OTK = V32[:, :, :].rearrange("p (t h) n -> p t (h n)", h=2)
        V32K = [("V32", kc) for kc in range(KC)]
        s.dma("sp", Wo[:], g["wout_b"][l], writes=["Wo"], after=g["wtok"](g["wout_b"], l))
        s.dma("sp", SELM[:], g["selm_in"][:, :, :], writes=["SELM"])
        wrr = {"w13": 0, "w2": 0, "hb": 0}
        act = [(ci, pos, N, isctx) for ci, (pos, N, isctx) in enumerate(chunks) if not (isctx and not need_ctx)]

        def XK(i):
            return [("Xr", i % 2, kc) for kc in range(KC)]

        def A_mix(i):
            ci, pos, N, isctx = act[i]
            s.dma("sp", MIX[:, 0:2, 0:N], fm(g["aT_s"], 2)[:, :, pos:pos + N], writes=["MIX"])
            s.dma("sp", MIX[:, 2:5, 0:N], fm(g["bT_s"], 3)[:, :, pos:pos + N], writes=["MIX"])
            s.dma("sp", MIX[:, 5:8, 0:N], fm(g["mT_s"], 3)[:, :, pos:pos + N], writes=["MIX"])

        def A_x(i):
            ci, pos, N, isctx = act[i]
            s.dma("sp", XR[i % 2][:, :, 0:N], fm(g["xT_s"], KC)[:, :, pos:pos + N], reads=[("xTw", ci)], writes=XK(i))

        def B(i):
            ci, pos, N, isctx = act[i]
            bi = 2 if isctx else b
            Xr = XR[i % 2]
            for oc in range(KC):
                bk = pb.next()
                for kc in range(KC):
                    s.op("pe", lambda: nc.tensor.matmul(pb.bank(bk)[:, 0:N], Wo[:, kc, oc * 128:(oc + 1) * 128], MIX[:, kc, 0:N],
                                                        start=(kc == 0), stop=(kc == KC - 1)),
                         reads=["Wo", "MIX"], writes=[bkey(bk)], psum=[bkey(bk)], signal=(kc == KC - 1))
                s.op("dve", lambda: nc.vector.scalar_tensor_tensor(out=Xr[:, oc, 0:N], in0=pb.bank(bk)[:, 0:N], scalar=modcol(l, 2, oc, bi),
                                                                    in1=Xr[:, oc, 0:N], op0=ALU.mult, op1=ALU.add),
                     reads=[bkey(bk), "mod", ("Xr", i % 2, oc)], writes=[("Xr", i % 2, oc)], psum=[bkey(bk)])

        def ln_stats(i):
            ci, pos, N, isctx = act[i]
            Xr = XR[i % 2]
            bk_m = pb.next()
            for kc in range(KC):
                s.op("pe", lambda: nc.tensor.matmul(pb.bank(bk_m)[:, 0:N], ones_f[:, :], Xr[:, kc, 0:N],
                                                    start=(kc == 0), stop=(kc == KC - 1)),
                     reads=["ones_f", ("Xr", i % 2, kc)], writes=[bkey(bk_m)], psum=[bkey(bk_m)], signal=(kc == KC - 1))
            bk_q = pb.next()
            for kc in range(KC):
                sq_ = SQ[kc % 2]
                sqk = ("SQ", kc % 2)
                if kc % 2 == 0:
                    s.op("act", lambda: nc.scalar.activation(out=sq_[:, 0:N], in_=Xr[:, kc, 0:N], func=AF.Square), reads=[("Xr", i % 2, kc)], writes=[sqk])
                else:
                    s.op("pool", lambda: nc.gpsimd.tensor_tensor(out=sq_[:, 0:N], in0=Xr[:, kc, 0:N], in1=Xr[:, kc, 0:N], op=ALU.mult),
                         reads=[("Xr", i % 2, kc)], writes=[sqk])
                s.op("pe", lambda: nc.tensor.matmul(pb.bank(bk_q)[:, 0:N], ones_r[:, :], sq_[:, 0:N],
                                                    start=(kc == 0), stop=(kc == KC - 1)),
                     reads=["ones_r", sqk], writes=[bkey(bk_q)], psum=[bkey(bk_q)], signal=True)
            mean = pb.bank(bk_m)
            ey2 = pb.bank(bk_q)
            m2, var, rs, nmr = ST[0], ST[1], ST[2], ST[3]
            s.op("act", lambda: nc.scalar.activation(out=m2[:, 0:N], in_=mean[:, 0:N], func=AF.Square), reads=[bkey(bk_m)], writes=["ST0"], psum=[bkey(bk_m)])
            s.op("dve", lambda: nc.vector.tensor_tensor(out=var[:, 0:N], in0=ey2[:, 0:N], in1=m2[:, 0:N], op=ALU.subtract),
                 reads=[bkey(bk_q), "ST0"], writes=["ST1"], psum=[bkey(bk_q)])
            s.op("act", lambda: nc.scalar.activation(out=var[:, 0:N], in_=var[:, 0:N], func=AF.Ln, bias=small[:, l, 39:40]), reads=["ST1", "small"], writes=["ST1"])
            s.op("act", lambda: nc.scalar.activation(out=rs[:, 0:N], in_=var[:, 0:N], func=AF.Exp, scale=-0.5), reads=["ST1"], writes=["ST2"])
            s.op("dve", lambda: nc.vector.scalar_tensor_tensor(out=nmr[:, 0:N], in0=mean[:, 0:N], scalar=-1.0, in1=rs[:, 0:N], op0=ALU.mult, op1=ALU.mult),
                 reads=[bkey(bk_m), "ST2"], writes=["ST3"], psum=[bkey(bk_m)])

        def ln_norm(i, gcol, bcol):
            ci, pos, N, isctx = act[i]
            Xr = XR[i % 2]
            rs, nmr = ST[2], ST[3]
            for kc in range(KC):
                e1 = "dve" if kc % 2 == 0 else "pool"
                eng1 = nc.vector if e1 == "dve" else nc.gpsimd
                xk = ("Xr", i % 2, kc)
                s.op(e1, lambda: eng1.tensor_tensor(out=Xr[:, kc, 0:N], in0=Xr[:, kc, 0:N], in1=rs[:, 0:N], op=ALU.mult), reads=[xk, "ST2"], writes=[xk])
                s.op(e1, lambda: eng1.tensor_tensor(out=Xr[:, kc, 0:N], in0=Xr[:, kc, 0:N], in1=nmr[:, 0:N], op=ALU.add), reads=[xk, "ST3"], writes=[xk])
                s.op("act", lambda: nc.scalar.activation(out=Xr[:, kc, 0:N], in_=Xr[:, kc, 0:N], func=AF.Identity,
                                                          scale=small[:, l, gcol + kc:gcol + kc + 1], bias=small[:, l, bcol + kc:bcol + kc + 1]),
                     reads=[xk, "small"], writes=[xk])

        def C2(i):
            ci, pos, N, isctx = act[i]
            bi = 2 if isctx else b
            Xr = XR[i % 2]
            ln_norm(i, 0, 8)
            for kc in range(KC):
                s.op("dve", lambda: nc.vector.tensor_scalar(out=V32[:, kc, 0:N], in0=Xr[:, kc, 0:N], scalar1=modcol(l, 4, kc, bi),
                                                             scalar2=modcol(l, 3, kc, bi), op0=ALU.mult, op1=ALU.add),
                     reads=[("Xr", i % 2, kc), "mod"], writes=[("V32", kc)])
                if kc % 2 == 0:
                    s.op("act", lambda: nc.scalar.copy(out=Vb[:, kc, 0:N], in_=V32[:, kc, 0:N]), reads=[("V32", kc)], writes=["Vb"])
                else:
                    s.op("pool", lambda: nc.gpsimd.tensor_copy(out=Vb[:, kc, 0:N], in_=V32[:, kc, 0:N]), reads=[("V32", kc)], writes=["Vb"])

        def C3a(i):
            ci, pos, N, isctx = act[i]
            nt = N // 128
            bk_r = pb.next()
            rbk = pb.bank(bk_r)
            for t in range(nt):
                for kc in range(KC):
                    s.op("pe", lambda: nc.tensor.matmul(rbk[:, t * NE:(t + 1) * NE], V32[:, kc, t * 128:(t + 1) * 128], router[:, kc, :],
                                                        start=(kc == 0), stop=(kc == KC - 1)),
                         reads=V32K + ["router"], writes=[bkey(bk_r)], psum=[bkey(bk_r)], signal=(kc == KC - 1))
            T4 = [128, nt, NE]
            s.op("act", lambda: nc.scalar.activation(out=SC[:, 0:nt, :], in_=rbk[:, 0:nt * NE].rearrange("p (t e) -> p t e", e=NE), func=AF.Sigmoid),
                 reads=[bkey(bk_r)], writes=["SC"], psum=[bkey(bk_r)])
            s.op("dve", lambda: nc.vector.tensor_tensor(out=SEL[:, 0:nt, :], in0=SC[:, 0:nt, :], in1=rbias[:, :].unsqueeze(1).to_broadcast(T4), op=ALU.add),
                 reads=["SC", "rbias"], writes=["SEL"])
            ng = nt * 4
            sel4 = SEL[:, 0:nt, :].rearrange("p t (g k) -> p (t g) k", k=4)
            sel24 = SEL2[:, 0:nt, :].rearrange("p t (g k) -> p (t g) k", k=4)
            eq4 = EQ[:, 0:nt, :].rearrange("p t (g k) -> p (t g) k", k=4)
            s.op("dve", lambda: nc.vector.tensor_reduce(out=M1[:, 0:ng], in_=sel4, axis=AX.X, op=ALU.max), reads=["SEL"], writes=["M1"])
            s.op("dve", lambda: nc.vector.tensor_tensor(out=eq4, in0=sel4, in1=M1[:, 0:ng].unsqueeze(2).to_broadcast([128, ng, 4]), op=ALU.is_equal),
                 reads=["SEL", "M1"], writes=["EQ"])
            s.op("dve", lambda: nc.vector.scalar_tensor_tensor(out=sel24, in0=eq4, scalar=-1e9, in1=sel4, op0=ALU.mult, op1=ALU.add),
                 reads=["EQ", "SEL"], writes=["SEL2"])
            s.op("dve", lambda: nc.vector.tensor_reduce(out=M2[:, 0:ng], in_=sel24, axis=AX.X, op=ALU.max), reads=["SEL2"], writes=["M2"])
            s.op("dve", lambda: nc.vector.tensor_tensor(out=GS[:, 0:ng], in0=M1[:, 0:ng], in1=M2[:, 0:ng], op=ALU.add), reads=["M1", "M2"], writes=["GS"])
            gs3 = GS[:, 0:ng].rearrange("p (t g) -> p t g", g=4)
            s.op("dve", lambda: nc.vector.tensor_reduce(out=GM[:, 0:nt], in_=gs3, axis=AX.X, op=ALU.max), reads=["GS"], writes=["GM"])
            s.op("dve", lambda: nc.vector.tensor_tensor(out=ING[:, 0:ng].rearrange("p (t g) -> p t g", g=4), in0=gs3,
                                                        in1=GM[:, 0:nt].unsqueeze(2).to_broadcast([128, nt, 4]), op=ALU.is_equal),
                 reads=["GS", "GM"], writes=["ING"])
            s.op("dve", lambda: nc.vector.tensor_tensor(out=eq4, in0=sel4, in1=M2[:, 0:ng].unsqueeze(2).to_broadcast([128, ng, 4]), op=ALU.is_ge),
                 reads=["SEL", "M2"], writes=["EQ"])
            s.op("dve", lambda: nc.vector.tensor_tensor(out=eq4, in0=eq4, in1=ING[:, 0:ng].unsqueeze(2).to_broadcast([128, ng, 4]), op=ALU.mult),
                 reads=["EQ", "ING"], writes=["EQ"])
            s.op("dve", lambda: nc.vector.tensor_tensor(out=GT[:, 0:nt, :], in0=EQ[:, 0:nt, :], in1=SC[:, 0:nt, :], op=ALU.mult),
                 reads=["EQ", "SC"], writes=["GT"])
            s.op("dve", lambda: nc.vector.tensor_reduce(out=DEN[:, 0:nt], in_=GT[:, 0:nt, :], axis=AX.X, op=ALU.add), reads=["GT"], writes=["DEN"])
            s.op("dve", lambda: nc.vector.reciprocal(out=DEN[:, 0:nt], in_=DEN[:, 0:nt]), reads=["DEN"], writes=["DEN"])
            s.op("dve", lambda: nc.vector.tensor_tensor(out=GT[:, 0:nt, :], in0=GT[:, 0:nt, :], in1=DEN[:, 0:nt].unsqueeze(2).to_broadcast(T4), op=ALU.mult),
                 reads=["GT", "DEN"], writes=["GT"])

        def C3b(i):
            ci, pos, N, isctx = act[i]
            nt = N // 128
            bk_t = pb.next()
            for t in range(nt):
                s.op("pe", lambda: nc.tensor.transpose(pb.bank(bk_t)[0:NE, t * 128:(t + 1) * 128], GT[:, t, :], ident[:]),
                     reads=["GT", "ident"], writes=[bkey(bk_t)], psum=[bkey(bk_t)], signal=(t == nt - 1))
            s.op("act", lambda: nc.scalar.copy(out=GTT[:, 0:N], in_=pb.bank(bk_t)[0:NE, 0:N]), reads=[bkey(bk_t)], writes=["GTT"], psum=[bkey(bk_t)])

        def E(i):
            ci, pos, N, isctx = act[i]
            for e in range(NE):
                wi = wrr["w13"] % 3
                wrr["w13"] += 1
                w13 = W13[wi]
                wk = ("W13", wi)
                s.dma("sp", w13[:], g["w13_b"][l, e], writes=[wk], after=[g["wtok"](g["w13_b"], l)[e]])
                bk_g = pb.next()
                s.op("pe", lambda: nc.tensor.matmul(pb.bank(bk_g)[:, 0:N], SELM[:, e, :], GTT[:, 0:N], start=True, stop=True),
                     reads=["SELM", "GTT"], writes=[bkey(bk_g)], psum=[bkey(bk_g)])
                for hc in range(2):
                    bk1 = pb.next()
                    for kc in range(KC):
                        s.op("pe", lambda: nc.tensor.matmul(pb.bank(bk1)[:, 0:N], w13[:, 0, kc, hc * 128:(hc + 1) * 128], Vb[:, kc, 0:N],
                                                            start=(kc == 0), stop=(kc == KC - 1)),
                             reads=[wk, "Vb"], writes=[bkey(bk1)], psum=[bkey(bk1)], signal=(kc == KC - 1))
                    bk3 = pb.next()
                    for kc in range(KC):
                        s.op("pe", lambda: nc.tensor.matmul(pb.bank(bk3)[:, 0:N], w13[:, 1, kc, hc * 128:(hc + 1) * 128], Vb[:, kc, 0:N],
                                                            start=(kc == 0), stop=(kc == KC - 1)),
                             reads=[wk, "Vb"], writes=[bkey(bk3)], psum=[bkey(bk3)], signal=(kc == KC - 1))
                    hi = wrr["hb"] % 2
                    wrr["hb"] += 1
                    s.op("act", lambda: nc.scalar.activation(out=SIL[hi][:, 0:N], in_=pb.bank(bk1)[:, 0:N], func=AF.Silu),
                         reads=[bkey(bk1)], writes=[("SIL", hi)], psum=[bkey(bk1)])
                    s.op("dve", lambda: nc.vector.tensor_tensor(out=HH[hi][:, 0:N], in0=pb.bank(bk3)[:, 0:N], in1=SIL[hi][:, 0:N], op=ALU.mult),
                         reads=[bkey(bk3), ("SIL", hi)], writes=[("HH", hi)], psum=[bkey(bk3)])
                    s.op("dve", lambda: nc.vector.tensor_tensor(out=GA[:, e, hc, 0:N], in0=pb.bank(bk_g)[:, 0:N], in1=HH[hi][:, 0:N], op=ALU.mult),
                         reads=[bkey(bk_g), ("HH", hi)], writes=[("GA", e, hc)], psum=[bkey(bk_g)])

        def Dh(i, half):
            ci, pos, N, isctx = act[i]
            bi = 2 if isctx else b
            Xr = XR[i % 2]
            for oc in range(half * 4, half * 4 + 4):
                wi = wrr["w2"] % 3
                wrr["w2"] += 1
                w2 = W2[wi]
                wk = ("W2", wi)
                s.dma("sp", w2[:], g["w2_b"][l, oc], writes=[wk], after=[g["wtok"](g["w2_b"], l)[oc]])
                bk = pb.next()
                for e in range(NE):
                    for hc in range(2):
                        s.op("pe", lambda: nc.tensor.matmul(pb.bank(bk)[:, 0:N], w2[:, e, hc, :], GA[:, e, hc, 0:N],
                                                            start=(e == 0 and hc == 0), stop=(e == NE - 1 and hc == 1)),
                             reads=[wk, ("GA", e, hc)], writes=[bkey(bk)], psum=[bkey(bk)], signal=(e == NE - 1 and hc == 1))
                s.op("dve", lambda: nc.vector.scalar_tensor_tensor(out=Xr[:, oc, 0:N], in0=pb.bank(bk)[:, 0:N], scalar=modcol(l, 5, oc, bi),
                                                                    in1=Xr[:, oc, 0:N], op0=ALU.mult, op1=ALU.add),
                     reads=[bkey(bk), "mod", ("Xr", i % 2, oc)], writes=[("Xr", i % 2, oc)], psum=[bkey(bk)])

        def D3(i):
            ci, pos, N, isctx = act[i]
            nt = N // 128
            Xr = XR[i % 2]
            ln_stats(i)
            ln_norm(i, 16, 24)
            if not last:
                s.dma("sp", fm(g["xT_s"], KC)[:, :, pos:pos + N], Xr[:, :, 0:N], reads=XK(i), writes=[("xTw", ci)])
            else:
                for t in range(nt):
                    for half in range(2):
                        bk = pb.next()
                        for j in range(4):
                            kc = half * 4 + j
                            s.op("pe", lambda: nc.tensor.transpose(pb.bank(bk)[:, j * 128:(j + 1) * 128], Xr[:, kc, t * 128:(t + 1) * 128], ident[:]),
                                 reads=[("Xr", i % 2, kc), "ident"], writes=[bkey(bk)], psum=[bkey(bk)], signal=(j == 3))
                        if half == 0:
                            s.op("act", lambda: nc.scalar.copy(out=OTK[:, t, 0:512], in_=pb.bank(bk)[:, :]), reads=[bkey(bk)], writes=V32K, psum=[bkey(bk)])
                        else:
                            s.op("dve", lambda: nc.vector.tensor_copy(out=OTK[:, t, 512:1024], in_=pb.bank(bk)[:, :]), reads=[bkey(bk)], writes=V32K, psum=[bkey(bk)])
                lp = pos - CTX
                s.dma("sp", g["out_d"][b, lp:lp + N, :].rearrange("(t p) d -> p t d", p=128), OTK[:, 0:nt, :], reads=V32K, writes=[("out", ci)])

        n = len(act)
        A_mix(0)
        A_x(0)
        for i in range(n):
            B(i)
            if i + 1 < n:
                A_mix(i + 1)
            ln_stats(i)
            C2(i)
            if i > 0:
                Dh(i - 1, 0)
            C3a(i)
            if i > 0:
                Dh(i - 1, 1)
            C3b(i)
            if i > 0:
                D3(i - 1)
            if i + 1 < n:
                A_x(i + 1)
            E(i)
        Dh(n - 1, 0)
        Dh(n - 1, 1)
        D3(n - 1)
        s.barrier()


def _swap32(a):
    sh = a.shape
    a = a.reshape(sh[:-1] + (sh[-1] // 32, 2, 16))
    a = a[..., ::-1, :]
    return np.ascontiguousarray(a.reshape(sh))


def _kc_layout(w):
    sh = w.shape
    w = w.reshape(sh[:-2] + (sh[-2] // 128, 128, sh[-1]))
    return np.ascontiguousarray(np.swapaxes(w, -3, -2))


def host_prepare(inp, lat, n_layers):
    f = np.float32
    L = n_layers
    out = {}
    w_in = np.asarray(inp["w_in"], f)[:L]
    ext = np.concatenate([w_in, _swap32(w_in[..., C_QDA:C_QDA + 384]), _swap32(w_in[..., C_KDA:C_KDA + 384]),
                          _swap32(w_in[..., C_KR:C_KR + 32])], axis=-1)
    out["w_in_r"] = _kc_layout(ext)
    out["w_out_r"] = _kc_layout(np.asarray(inp["w_out"], f)[:L])
    wuq = np.asarray(inp["mla_w_uq"], f)[:L].reshape(L, 256, 6, 96)
    nope = wuq[..., :64].reshape(L, 256, 384)
    rope = wuq[..., 64:].reshape(L, 256, 192)
    out["w_uq_r"] = _kc_layout(np.concatenate([nope, rope, _swap32(rope)], axis=-1))
    wukv = np.asarray(inp["mla_w_ukv"], f)[:L].reshape(L, 128, 6, 128)
    out["w_ukv_r"] = np.ascontiguousarray(np.concatenate([wukv[..., :64].reshape(L, 128, 384), wukv[..., 64:].reshape(L, 128, 384)], axis=-1))
    wa = np.asarray(inp["lru_wa"], f)[:L]
    wi = np.asarray(inp["lru_wi"], f)[:L]
    lw = np.zeros((L, 128, 8, 128), f)
    for d in range(2):
        for c in range(2):
            for ai, w in enumerate((wa, wi)):
                for hb in range(2):
                    lw[:, hb * 64:(hb + 1) * 64, d * 4 + c * 2 + ai, hb * 64:(hb + 1) * 64] = w[:, d, 2 * c + hb]
    out["lru_w_r"] = lw
    w1 = _kc_layout(np.asarray(inp["exp_w1"], f)[:L])
    w3 = _kc_layout(np.asarray(inp["exp_w3"], f)[:L])
    out["w13_r"] = np.ascontiguousarray(np.stack([w1, w3], axis=3))
    w2 = np.asarray(inp["exp_w2"], f)[:L].reshape(L, NE, 2, 128, 8, 128)
    out["w2_r"] = np.ascontiguousarray(w2.transpose(0, 4, 3, 1, 2, 5))
    wm = np.asarray(inp["w_mod"], f)[:L].reshape(L, 8, 128, 12, 512)
    out["w_mod_r"] = np.ascontiguousarray(wm.transpose(0, 3, 2, 1, 4))
    out["b_mod_r"] = np.ascontiguousarray(np.asarray(inp["b_mod"], f)[:L].reshape(L, 48, 128).transpose(2, 0, 1))
    out["router_r"] = _kc_layout(np.asarray(inp["router_w"], f))
    out["router_b_r"] = np.ascontiguousarray(np.broadcast_to(np.asarray(inp["router_b"], f)[None, :], (128, NE)))
    small = np.zeros((128, L, 40), f)

    def chunked(v, n):
        return np.asarray(v, f)[:L].reshape(L, n, 128).transpose(2, 0, 1)

    small[:, :, 0:8] = chunked(inp["ln1_g"], 8)
    small[:, :, 8:16] = chunked(inp["ln1_b"], 8)
    small[:, :, 16:24] = chunked(inp["ln2_g"], 8)
    small[:, :, 24:32] = chunked(inp["ln2_b"], 8)
    small[:, :, 32:34] = chunked(inp["mla_q_norm"], 2)
    small[:, :, 34:35] = chunked(inp["mla_kv_norm"], 1)
    small[:, :, 35:37] = chunked(inp["conv_b"], 2)
    dn = np.asarray(inp["diff_norm"], f)[:L]
    small[:, :, 37] = np.concatenate([dn, dn], axis=1).T
    small[:, :, 38] = RMS_EPS
    small[:, :, 39] = LN_EPS_EFF
    out["small_r"] = small
    small2 = np.zeros((128, L, 16), f)
    cw = np.asarray(inp["conv_w"], f)[:L].reshape(L, 4, 2, 128)
    small2[:, :, 0:8] = cw.transpose(3, 0, 1, 2).reshape(128, L, 8)
    for nm, o in (("lru_ba", 8), ("lru_bi", 12)):
        v = np.asarray(inp[nm], f)[:L].reshape(L, 2, 2, 128)
        small2[:, :, o:o + 4] = v.transpose(3, 0, 1, 2).reshape(128, L, 4)
    out["small2_r"] = small2
    lam = np.asarray(inp["lru_lambda"], f)[:L].reshape(L, 2, 2, 128)
    out["lam_r"] = np.ascontiguousarray(lam.transpose(3, 0, 1, 2).reshape(128, L, 4))
    out["dlam_r"] = np.ascontiguousarray(np.broadcast_to(np.asarray(inp["diff_lambda"], f)[:L][None], (128, L, 4, 32)))
    t = np.arange(lat)
    row = (t // GRID_W).astype(np.float64)
    col = (t % GRID_W).astype(np.float64)
    inv = 10000.0 ** (-np.arange(8, dtype=np.float64) / 8)
    ang = np.concatenate([row[:, None] * inv, col[:, None] * inv], axis=-1)
    ang32 = np.concatenate([row.astype(f)[:, None] * inv.astype(f), col.astype(f)[:, None] * inv.astype(f)], axis=-1).astype(f)
    cos = np.cos(ang32).astype(f)
    sin = np.sin(ang32).astype(f)
    cosT = np.zeros((128, lat), f)
    sinT = np.zeros((128, lat), f)
    for p in range(128):
        j = p % 32
        cosT[p] = cos[:, j % 16]
        sinT[p] = -sin[:, j % 16] if j < 16 else sin[:, j % 16]
    out["cosT"] = cosT
    out["sinT"] = sinT
    out["ident"] = np.eye(128, dtype=f)
    import ml_dtypes
    selm = np.zeros((16, NE, 128), ml_dtypes.bfloat16)
    for e in range(NE):
        selm[e, e, :] = 1.0
    out["selm"] = selm
    return out


def kernel(**inputs):
    return run_kernel(inputs)


def run_kernel(inputs, lat=4096, n_layers=DEPTH, n_cores=8, nb=2, debug=(), stop_after=None):
    shared = host_prepare(inputs, lat, n_layers)
    x = np.asarray(inputs["x"], np.float32)
    ctx = np.asarray(inputs["ctx"], np.float32)
    c = np.asarray(inputs["c"], np.float32)
    c_ctx = np.asarray(inputs["c_ctx"], np.float32)
    nc = build_program(lat=lat, n_layers=n_layers, nb=nb, debug=debug, stop_after=stop_after)
    in_maps = []
    for core in range(n_cores):
        m = dict(shared)
        bs = slice(core * nb, (core + 1) * nb)
        m["x"] = np.ascontiguousarray(x[bs])
        m["ctx"] = np.ascontiguousarray(ctx[bs])
        cT = np.zeros((128, KC, 4), np.float32)
        for j in range(nb):
            cT[:, :, j] = c[core * nb + j].reshape(KC, 128).T
        cT[:, :, 2] = c_ctx.reshape(KC, 128).T
        m["cT"] = cT
        in_maps.append(m)
    res = run_bass_kernel_spmd(nc, in_maps, core_ids=list(range(n_cores)))
    out = np.concatenate([np.asarray(r["out"]) for r in res.results], axis=0)
    if debug:
        return out, res.results
    return out
```
